# Optimizing a Trainium2 kernel written in Bass

```python
import math
import jax, jax.numpy as jnp
from jax import lax
import numpy as np

D_MODEL = 1024
BATCH = 2
SEQ = 16384
DEPTH = 1

PLE_DIM = 256
MOBA_HEADS = 8
MOBA_HEAD_DIM = D_MODEL // 16
MOBA_WIDTH = MOBA_HEADS * MOBA_HEAD_DIM
MOBA_BLOCK = 256
MOBA_TOPK = 3
MOBA_Q_CHUNK = 64
RET_HEADS = 4
RET_HEAD_DIM = D_MODEL // 8
RET_WIDTH = RET_HEADS * RET_HEAD_DIM
RET_CHUNK = 256
MIX_WIDTH = MOBA_WIDTH + RET_WIDTH
IN_PROJ_WIDTH = 3 * MOBA_WIDTH + 4 * RET_WIDTH
SEQ_PAD_MULTIPLE = 256
D_FF = 2816
CONV_WIDTH = 3
RMS_EPS = 1e-6
GN_EPS = 1e-5

kernel_name = "hymba_moba_retnet_convffn_ple"


def rmsnorm(x, g):
    xf = x.astype(jnp.float32)
    ms = jnp.mean(xf * xf, axis=-1, keepdims=True)
    return (xf * lax.rsqrt(ms + RMS_EPS)).astype(x.dtype) * g


def alibi_slopes(n_heads):
    return jnp.exp2(-8.0 * jnp.arange(1, n_heads + 1, dtype=jnp.float32) / n_heads)


def to_heads(t, n_heads, head_dim):
    b, s, _ = t.shape
    return t.reshape(b, s, n_heads, head_dim).transpose(0, 2, 1, 3)


def from_heads(t):
    b, h, s, d = t.shape
    return t.transpose(0, 2, 1, 3).reshape(b, s, h * d)


def moba_attention(q, k, v):
    B, H, S, Dh = q.shape
    nb = S // MOBA_BLOCK
    k_eff = min(MOBA_TOPK, nb)
    n_sel = k_eff * MOBA_BLOCK
    scale = Dh ** -0.5
    slopes = alibi_slopes(H)[:, None, None]
    kb = k.reshape(B, H, nb, MOBA_BLOCK, Dh)
    vb = v.reshape(B, H, nb, MOBA_BLOCK, Dh)
    kmean = jnp.mean(kb.astype(jnp.float32), axis=3).astype(k.dtype)
    kb_flat = kb.reshape(B * H * nb, MOBA_BLOCK, Dh)
    vb_flat = vb.reshape(B * H * nb, MOBA_BLOCK, Dh)
    head_base = (jnp.arange(B * H, dtype=jnp.int32) * nb).reshape(B, H, 1, 1)
    chunks_per_block = MOBA_BLOCK // MOBA_Q_CHUNK
    blk_pos = jnp.arange(MOBA_BLOCK, dtype=jnp.int32)
    blk_ids = jnp.arange(nb, dtype=jnp.int32)

    def one_chunk(c):
        start = c * MOBA_Q_CHUNK
        qc = lax.dynamic_slice_in_dim(q, start, MOBA_Q_CHUNK, axis=2)
        own = c // chunks_per_block
        q_pos = start + jnp.arange(MOBA_Q_CHUNK, dtype=jnp.int32)
        gate = jnp.einsum('bhqd,bhnd->bhqn', qc, kmean).astype(jnp.float32)
        gate = jnp.where(blk_ids < own, gate, -jnp.inf)
        gate_val, sel = lax.top_k(gate, k_eff)
        sel_valid = jnp.isfinite(gate_val)
        flat = head_base + sel
        k_sel = kb_flat[flat].reshape(B, H, MOBA_Q_CHUNK, n_sel, Dh)
        v_sel = vb_flat[flat].reshape(B, H, MOBA_Q_CHUNK, n_sel, Dh)
        s_sel = jnp.einsum('bhqd,bhqkd->bhqk', qc, k_sel).astype(jnp.float32) * scale
        key_pos_sel = (sel[..., None] * MOBA_BLOCK + blk_pos).reshape(B, H, MOBA_Q_CHUNK, n_sel)
        dist_sel = (q_pos[:, None] - key_pos_sel).astype(jnp.float32)
        s_sel = jnp.where(jnp.repeat(sel_valid, MOBA_BLOCK, axis=-1),
                          s_sel - slopes * dist_sel, -jnp.inf)
        k_own = lax.dynamic_index_in_dim(kb, own, axis=2, keepdims=False)
        v_own = lax.dynamic_index_in_dim(vb, own, axis=2, keepdims=False)
        s_own = jnp.einsum('bhqd,bhkd->bhqk', qc, k_own).astype(jnp.float32) * scale
        dist_own = q_pos[:, None] - (own * MOBA_BLOCK + blk_pos)[None, :]
        s_own = jnp.where(dist_own >= 0, s_own - slopes * dist_own.astype(jnp.float32), -jnp.inf)
        probs = jax.nn.softmax(jnp.concatenate([s_sel, s_own], axis=-1), axis=-1).astype(v.dtype)
        out = (jnp.einsum('bhqk,bhqkd->bhqd', probs[..., :n_sel], v_sel)
               + jnp.einsum('bhqk,bhkd->bhqd', probs[..., n_sel:], v_own))
        return out

    outs = lax.map(one_chunk, jnp.arange(S // MOBA_Q_CHUNK, dtype=jnp.int32))
    return jnp.moveaxis(outs, 0, 2).reshape(B, H, S, Dh)


def retention_chunkwise(q, k, v):
    B, H, S, Dh = q.shape
    C = RET_CHUNK
    nc = S // C
    gamma = 1.0 - jnp.exp2(-5.0 - jnp.arange(H, dtype=jnp.float32))
    log_g = jnp.log(gamma)
    idx = jnp.arange(C, dtype=jnp.float32)
    diff = idx[:, None] - idx[None, :]
    decay_intra = jnp.where(diff >= 0, jnp.exp(log_g[:, None, None] * jnp.maximum(diff, 0.0)), 0.0)
    q_decay = jnp.exp(log_g[:, None] * (idx + 1.0))[..., None]
    k_decay = jnp.exp(log_g[:, None] * (C - 1.0 - idx))[..., None]
    chunk_decay = jnp.exp(log_g * C)[:, None, None]
    k = k * (Dh ** -0.5)

    def chunks(t):
        return jnp.moveaxis(t.reshape(B, H, nc, C, Dh), 2, 0)

    def step(state, inp):
        qc, kc, vc = inp
        s = jnp.einsum('bhqd,bhkd->bhqk', qc, kc) * decay_intra
        inner = jnp.einsum('bhqk,bhkd->bhqd', s, vc)
        cross = jnp.einsum('bhqd,bhde->bhqe', qc * q_decay, state)
        new_state = chunk_decay * state + jnp.einsum('bhkd,bhke->bhde', kc * k_decay, vc)
        return new_state, inner + cross

    state0 = jnp.zeros((B, H, Dh, Dh), jnp.float32)
    _, outs = lax.scan(step, state0, (chunks(q), chunks(k), chunks(v)))
    return jnp.moveaxis(outs, 0, 2).reshape(B, H, S, Dh)


def head_groupnorm(o, g):
    mu = jnp.mean(o, axis=-1, keepdims=True)
    var = jnp.mean(jnp.square(o - mu), axis=-1, keepdims=True)
    return from_heads((o - mu) * lax.rsqrt(var + GN_EPS)) * g


def causal_dwconv(u, w, b):
    S = u.shape[1]
    up = jnp.pad(u, ((0, 0), (CONV_WIDTH - 1, 0), (0, 0)))
    out = b
    for j in range(CONV_WIDTH):
        out = out + w[j] * up[:, j:j + S]
    return out


def setup_inputs(seed: int = 0) -> dict:
    key = jax.random.key(seed)
    ks = jax.random.split(key, 17)
    f32 = jnp.float32

    def nrm(k, shape, scale):
        return jax.random.normal(k, shape, f32) * scale

    def gain(k, shape):
        return 1.0 + 0.02 * jax.random.normal(k, shape, f32)

    return {
        "x": nrm(ks[0], (BATCH, SEQ, D_MODEL), 1.0),
        "p": nrm(ks[1], (DEPTH, BATCH, SEQ, PLE_DIM), 1.0),
        "attn_norm": gain(ks[2], (DEPTH, D_MODEL)),
        "w_in": nrm(ks[3], (DEPTH, D_MODEL, IN_PROJ_WIDTH), D_MODEL ** -0.5),
        "ret_norm": gain(ks[4], (DEPTH, RET_WIDTH)),
        "w_out": nrm(ks[5], (DEPTH, MIX_WIDTH, D_MODEL), MIX_WIDTH ** -0.5),
        "ffn_norm": gain(ks[6], (DEPTH, D_MODEL)),
        "w_up": nrm(ks[7], (DEPTH, D_MODEL, D_FF), D_MODEL ** -0.5),
        "w_gate": nrm(ks[8], (DEPTH, D_MODEL, D_FF), D_MODEL ** -0.5),
        "conv_w": nrm(ks[9], (DEPTH, CONV_WIDTH, D_FF), CONV_WIDTH ** -0.5),
        "conv_b": nrm(ks[10], (DEPTH, D_FF), 0.01),
        "w_down": nrm(ks[11], (DEPTH, D_FF, D_MODEL), D_FF ** -0.5),
        "ple_norm": gain(ks[12], (DEPTH, D_MODEL)),
        "w_ple": nrm(ks[13], (DEPTH, PLE_DIM, D_MODEL), PLE_DIM ** -0.5),
        "w_ple_gate": nrm(ks[14], (DEPTH, D_MODEL, D_MODEL), D_MODEL ** -0.5),
        "final_norm": gain(ks[15], (D_MODEL,)),
    }


def reference(x, p, attn_norm, w_in, ret_norm, w_out, ffn_norm, w_up, w_gate, conv_w,
              conv_b, w_down, ple_norm, w_ple, w_ple_gate, final_norm):
    B, S, _ = x.shape
    S_pad = -(-S // SEQ_PAD_MULTIPLE) * SEQ_PAD_MULTIPLE
    split_points = [MOBA_WIDTH, 2 * MOBA_WIDTH, 3 * MOBA_WIDTH,
                    3 * MOBA_WIDTH + RET_WIDTH, 3 * MOBA_WIDTH + 2 * RET_WIDTH,
                    3 * MOBA_WIDTH + 3 * RET_WIDTH]
    for i in range(DEPTH):
        h = rmsnorm(x, attn_norm[i])
        proj = h @ w_in[i]
        proj = jnp.pad(proj, ((0, 0), (0, S_pad - S), (0, 0)))
        mq, mk, mv, rq, rk, rv, rg = jnp.split(proj, split_points, axis=-1)
        moba_out = from_heads(moba_attention(to_heads(mq, MOBA_HEADS, MOBA_HEAD_DIM),
                                             to_heads(mk, MOBA_HEADS, MOBA_HEAD_DIM),
                                             to_heads(mv, MOBA_HEADS, MOBA_HEAD_DIM)))
        ret = retention_chunkwise(to_heads(rq, RET_HEADS, RET_HEAD_DIM),
                                  to_heads(rk, RET_HEADS, RET_HEAD_DIM),
                                  to_heads(rv, RET_HEADS, RET_HEAD_DIM))
        ret_out = (head_groupnorm(ret, ret_norm[i]) * jax.nn.silu(rg)).astype(x.dtype)
        mix = jnp.concatenate([moba_out, ret_out], axis=-1)[:, :S]
        x = x + mix @ w_out[i]
        h = rmsnorm(x, ffn_norm[i])
        u = causal_dwconv(h @ w_up[i], conv_w[i], conv_b[i])
        x = x + (jax.nn.gelu(u) * (h @ w_gate[i])) @ w_down[i]
        g = jax.nn.sigmoid(rmsnorm(x, ple_norm[i]) @ w_ple_gate[i])
        x = x + (p[i] @ w_ple[i]) * g
    return rmsnorm(x, final_norm)
```

```python
import contextlib
import numpy as np
import ml_dtypes
import concourse.bass as bass
import concourse.mybir as mybir
from concourse.bass_utils import run_bass_kernel_spmd

F32 = mybir.dt.float32
BF16 = mybir.dt.bfloat16
ALU = mybir.AluOpType
AF = mybir.ActivationFunctionType
AX = mybir.AxisListType

S = 16384
D = 1024
NB = 64
NG = 32
NSEG = 8
SEGT = 2048
TQ = 4096
NT2 = 8
FF = 2816
NF = 22
BIG = 32768.0
WIN0 = 5
RMS_EPS = 1e-6
GN_EPS = 1e-5

DEBUG = {}


class Sem:
    def __init__(self, nc, name):
        self.h = nc.alloc_semaphore(name)
        self.v = 0


class Buf:
    __slots__ = ("name", "w", "r", "excl", "dsem")

    def __init__(self, name, excl=False):
        self.name = name
        self.w = None
        self.r = []
        self.excl = excl
        self.dsem = None


class Ctx:
    def __init__(self, nc):
        self.nc = nc
        self.eng = {"pe": nc.tensor, "act": nc.scalar, "dve": nc.vector, "pool": nc.gpsimd, "sp": nc.sync}
        self.esem = {k: Sem(nc, "e_" + k) for k in self.eng}
        self.waited = {k: {} for k in self.eng}
        self.nsem = 0
        self.dma_toks = []

    def newsem(self, name):
        self.nsem += 1
        return Sem(self.nc, "%s_%d" % (name, self.nsem))

    def wait(self, e, tok):
        sem, val = tok[0], tok[1]
        cur = self.waited[e].get(id(sem), 0)
        if cur >= val:
            return
        self.eng[e].wait_ge(sem.h, val)
        self.waited[e][id(sem)] = val

    def deps(self, e, reads, writes):
        toks = []
        for b in reads:
            if b.w is not None:
                toks.append(b.w)
            if b.excl:
                toks += [t for t in b.r if t[2] != e]
        for b in writes:
            if b.w is not None and (b.w[2] != e):
                toks.append(b.w)
            elif b.w is not None and b.excl and e != "pe":
                toks.append(b.w)
            toks += [t for t in b.r if t[2] != e]
        for t in toks:
            self.wait(e, t)

    def mark(self, tok, reads, writes):
        for b in reads:
            b.r.append(tok)
        for b in writes:
            b.w = tok
            b.r = []

    def op(self, e, fn, reads=(), writes=()):
        self.deps(e, reads, writes)
        ins = fn(self.eng[e])
        s = self.esem[e]
        s.v += 1
        ins.then_inc(s.h, 1)
        tok = (s, s.v, e)
        self.mark(tok, reads, writes)
        return tok

    def dma(self, q, out, in_, reads=(), writes=(), sem=None):
        self.deps(q, reads, writes)
        if sem is None:
            owner = None
            for b in list(writes) + list(reads):
                owner = b
                break
            if owner.dsem is None:
                owner.dsem = self.newsem("d")
            sem = owner.dsem
        ins = self.eng[q].dma_start(out=out, in_=in_)
        sem.v += 16
        ins.then_inc(sem.h, 16)
        tok = (sem, sem.v, None)
        self.mark(tok, reads, writes)
        self.dma_toks.append(tok)
        return tok

    def barrier(self):
        for e in self.eng:
            for e2, s in self.esem.items():
                if e2 != e and s.v > 0:
                    self.wait(e, (s, s.v, e2))
            for t in self.dma_toks:
                self.wait(e, t)
        self.dma_toks = []


class Rot:
    def __init__(self, items):
        self.items = items
        self.i = 0

    def next(self):
        it = self.items[self.i % len(self.items)]
        self.i += 1
        return it


def build_program():
    nc = bass.Bass("TRN2", target_bir_lowering=False)
    K = Ctx(nc)
    es = contextlib.ExitStack()

    def din(name, shape, dt=F32):
        return nc.dram_tensor(name, list(shape), dt, kind="ExternalInput").ap()

    xb = din("xb", [S, D])
    pq = din("pq", [TQ, 256])
    xq = din("xq", [TQ, D])
    xh = din("xh", [2, D])
    w1 = din("w1", [D, 1024])
    an = din("an", [128, 8])
    wout = din("wout", [D, D])
    wup = din("wup", [D, FF])
    wgate = din("wgate", [D, FF])
    wdown = din("wdown", [FF, D])
    wple = din("wple", [256, D])
    wpg = din("wpg", [D, D])
    fnorm = din("fn", [128, 8])
    pnorm = din("pn", [128, 8])
    cw = din("cw", [128, NF * 3])
    cb = din("cb", [128, NF])
    fin = din("fin", [128, D])
    rn = din("rn", [128, 128])
    hflag = din("hflag", [128, 1])
    c_kst = din("c_kst", [64, S], BF16)
    c_dtab = din("c_dtab", [128, 2 * 128])
    c_negr = din("c_negr", [128, 4])
    c_bdiag = din("c_bdiag", [128, 2 * 2 * 256], BF16)
    c_ekb = din("c_ekb", [128, 4])
    c_decT = din("c_decT", [128, 2 * 256])
    c_qdec = din("c_qdec", [128, 512])
    c_kdec = din("c_kdec", [128, 2])
    c_cd = din("c_cd", [128, 1])
    c_idb = din("c_idb", [128, 128], BF16)
    y = nc.dram_tensor("y", [TQ, D], F32, kind="ExternalOutput").ap()
    dbg = {}
    if DEBUG.get("mix"):
        dbg["mix"] = nc.dram_tensor("dbg_mix", [256, S], BF16, kind="ExternalOutput").ap()

    agi = [nc.dram_tensor("agi%d" % k, [256, SEGT], BF16) for k in range(NSEG)]
    ago = [nc.dram_tensor("ago%d" % k, [1024, SEGT], BF16) for k in range(NSEG)]
    mine = nc.dram_tensor("mine", [1024, NSEG * SEGT], BF16)
    wupg_s = nc.dram_tensor("wupg_s", [NF, 2, 128, 8 * 128], BF16)
    wdown_s = nc.dram_tensor("wdown_s", [128, NF * D], BF16)
    wout_s = nc.dram_tensor("wout_s", [128, 8 * D], BF16)
    wpg_s = nc.dram_tensor("wpg_s", [128, 8 * D], BF16)
    wple_s = nc.dram_tensor("wple_s", [128, 2 * D], BF16)

    def sb(name, shape, dt=F32):
        return es.enter_context(nc.sbuf_tensor(name, list(shape), dt))

    def ps(name, shape, dt=F32):
        return es.enter_context(nc.psum_tensor(name, list(shape), dt))

    trb = [(ps("trb%d" % i, [128, 1024], BF16), Buf("trb%d" % i, True)) for i in range(1)]
    fpb = [(ps("fpb%d" % i, [128, 512], F32), Buf("fpb%d" % i, True)) for i in range(5)]
    obk = [(ps("obk%d" % i, [128, 512], F32), Buf("obk%d" % i, True)) for i in range(2)]
    TR = Rot(trb)
    FP = Rot(fpb[0:2])
    AFP = Rot(fpb[2:5])
    FP2 = Rot(fpb)

    idb = sb("idb", [128, 128], BF16)
    epsr = sb("epsr", [128, 1])
    epsg = sb("epsg", [128, 1])
    zero2 = sb("zero2", [128, 2], BF16)
    K.dma("sp", idb[:], c_idb[:, :], writes=[Buf("c")])
    K.op("dve", lambda e: e.memset(epsr[:], RMS_EPS))
    K.op("dve", lambda e: e.memset(epsg[:], GN_EPS))
    K.op("dve", lambda e: e.memset(zero2[:], 0.0))

    gains = sb("gains", [128, 16])
    gB = Buf("gains")
    K.dma("sp", gains[:, 0:8], fnorm[:, :], writes=[gB])
    K.dma("sp", gains[:, 8:16], pnorm[:, :], writes=[gB])

    with contextlib.ExitStack() as p1:
        def s1(name, shape, dt=F32):
            return p1.enter_context(nc.sbuf_tensor(name, list(shape), dt))

        W1b = s1("W1b", [128, 8, 1024], BF16)
        KA = [s1("KaugA", [128, S], BF16), s1("KaugB", [128, S], BF16)]
        VV = s1("VV", [128, 128, 192], BF16)
        KAB = [[Buf("KaugA%d" % i) for i in range(NG)], [Buf("KaugB%d" % i) for i in range(NG)]]
        VAB = [Buf("VV%d" % i) for i in range(NG)]
        kmT = [s1("kmA", [64, 64], BF16), s1("kmB", [64, 64], BF16)]
        kmB_ = [Buf("kmA"), Buf("kmB")]
        Ctab = s1("Ctab", [128, 128])
        dtab = s1("dtab", [128, 256])
        negr = s1("negr", [128, 4])
        bdiag = s1("bdiag", [128, 2, 2, 256], BF16)
        ekb = s1("ekb", [128, 4])
        decT = s1("decT", [128, 2, 256])
        qdec = s1("qdec", [128, 512])
        kdec = s1("kdec", [128, 2])
        cdc = s1("cdc", [128, 1])
        rng_ = s1("rng", [128, 128])
        an_s = s1("an_s", [128, 8])
        cB = Buf("consts")
        for dst, src in ((dtab[:], c_dtab[:, :]), (negr[:], c_negr[:, :]),
                         (bdiag[:].rearrange("p a b c -> p (a b c)"), c_bdiag[:, :]),
                         (ekb[:], c_ekb[:, :]), (decT[:].rearrange("p a b -> p (a b)"), c_decT[:, :]),
                         (qdec[:], c_qdec[:, :]), (kdec[:], c_kdec[:, :]), (cdc[:], c_cd[:, :]),
                         (rng_[:], rn[:, :]), (an_s[:], an[:, :])):
            K.dma("sp", dst, src, writes=[cB])
        for X in range(2):
            K.dma("sp", KA[X][64:128, :], c_kst[:, :], writes=[cB])
        K.op("dve", lambda e: e.memset(Ctab[:, 0:64], 0.0))
        K.op("dve", lambda e: e.memset(Ctab[:, 64:128], -1e30))
        K.op("pool", lambda e: e.memset(VV[:, :, 64:128], 1.0))
        for X in range(2):
            K.op("dve", lambda e, X=X: e.memset(kmT[X][:], 0.0))
        with contextlib.ExitStack() as pes:
            wst = [(pes.enter_context(nc.sbuf_tensor("wst%d" % i, [128, 1024], F32)), Buf("wst%d" % i)) for i in range(2)]
            for c in range(8):
                st, stB = wst[c % 2]
                K.dma("sp", st[:], w1[c * 128:(c + 1) * 128, :], writes=[stB])
                K.op("dve", lambda e, st=st, c=c: e.tensor_scalar(out=W1b[:, c, :], in0=st[:], scalar1=an_s[:, c:c + 1],
                                                                  scalar2=None, op0=ALU.mult), reads=[stB, cB])
            K.barrier()

        mbin = [[[(s1("mbin%d%d%d" % (X, par, i), [128, 128], BF16), Buf("mbin")) for i in range(2)]
                 for par in range(2)] for X in range(2)]
        for X in range(2):
            for par in range(2):
                for i in range(2):
                    t_, _ = mbin[X][par][i]
                    K.op("dve", lambda e, t_=t_: e.memset(t_[:, 0:64], 0.0))
                    K.op("dve", lambda e, t_=t_, X=X, par=par: e.tensor_copy(out=t_[:, 127:128],
                                                                             in_=negr[:, 2 * X + par:2 * X + par + 1]))
        xt = [(s1("xt%d" % i, [128, 1024]), Buf("xt%d" % i)) for i in range(2)]
        ssb = [(s1("ss%d" % i, [128, 1]), Buf("ss")) for i in range(4)]
        rsb = [(s1("rs%d" % i, [128, 1]), Buf("rs")) for i in range(4)]
        xs = [(s1("xs%d" % i, [128, 1024], BF16), Buf("xs%d" % i)) for i in range(2)]
        hT = [(s1("hT%d" % i, [128, 8, 512], BF16), Buf("hT%d" % i)) for i in range(2)]
        QA = [[(s1("Qaug%d%d" % (X, i), [128, 512], BF16), Buf("Qaug")) for i in range(2)] for X in range(2)]
        kms = [(s1("kms%d" % i, [128, 2]), Buf("kms")) for i in range(2)]
        Gm = [(s1("Gm%d" % i, [128, 64]), Buf("Gm")) for i in range(2)]
        m8 = [(s1("m8%d" % i, [128, 8]), Buf("m8")) for i in range(2)]
        thr = [(s1("thr%d" % i, [128, 1]), Buf("thr")) for i in range(2)]
        mb32 = [(s1("mb32%d" % i, [128, 64]), Buf("mb32")) for i in range(2)]
        PT = Rot([(s1("PT%d" % i, [128, 512], BF16), Buf("PT%d" % i)) for i in range(3)])
        rc0 = (s1("rc0", [128, 512]), Buf("rc"))
        rc = [rc0, rc0]
        mixst = [(s1("mixst%d" % i, [128, 512], BF16), Buf("mixst%d" % i)) for i in range(2)]
        retst = [(s1("retst%d" % i, [128, 512], BF16), Buf("retst%d" % i)) for i in range(2)]
        rqT = [(s1("rqT%d" % i, [128, 512], BF16), Buf("rqT")) for i in range(2)]
        rqTd = [(s1("rqTd%d" % i, [128, 512], BF16), Buf("rqTd")) for i in range(2)]
        rkT = [(s1("rkT%d" % i, [128, 512], BF16), Buf("rkT")) for i in range(2)]
        rkd = [(s1("rkd%d" % i, [128, 4, 128], BF16), Buf("rkd")) for i in range(2)]
        rvt = [(s1("rvt%d" % i, [128, 4, 128], BF16), Buf("rvt")) for i in range(2)]
        sg = [(s1("sg%d" % i, [128, 4, 128]), Buf("sg")) for i in range(2)]
        gs = sg
        AT = Rot([(s1("AT%d" % i, [128, 2, 256], BF16), Buf("AT")) for i in range(2)])
        st6 = Rot([(s1("st6%d" % i, [128, 6]), Buf("st6")) for i in range(2)])
        mv = Rot([(s1("mv%d" % i, [128, 2]), Buf("mv")) for i in range(2)])
        rstd2 = Rot([(s1("rstd2%d" % i, [128, 1]), Buf("rstd2")) for i in range(2)])
        yn = Rot([(s1("yn%d" % i, [128, 128]), Buf("yn")) for i in range(2)])
        ynb = Rot([(s1("ynb%d" % i, [128, 128], BF16), Buf("ynb")) for i in range(2)])
        state = s1("state", [128, 128])
        stateB = Buf("state")
        stbf = Rot([(s1("stbf%d" % i, [128, 128], BF16), Buf("stbf")) for i in range(2)])
        K.op("dve", lambda e: e.memset(state[:], 0.0), writes=[stateB])
        cur_st = stbf.next()
        K.op("dve", lambda e: e.memset(cur_st[0][:], 0.0), writes=[cur_st[1]])
        K.barrier()

        ALT = [0]

        def evac_eng():
            ALT[0] += 1
            return "act" if ALT[0] % 2 else "dve"

        def copy_op(eng, out, in_, reads, writes, scale=None):
            if eng == "act":
                if scale is None:
                    return K.op("act", lambda e: e.copy(out=out, in_=in_), reads=reads, writes=writes)
                return K.op("act", lambda e: e.mul(out=out, in_=in_, mul=scale), reads=reads, writes=writes)
            if scale is None:
                return K.op(eng, lambda e: e.tensor_copy(out=out, in_=in_), reads=reads, writes=writes)
            return K.op(eng, lambda e: e.tensor_scalar(out=out, in0=in_, scalar1=scale, scalar2=None, op0=ALU.mult),
                        reads=reads, writes=writes)

        def prep(g):
            h, hB = hT[g % 2]
            for t in range(4):
                T = 4 * g + t
                x_, xB = xt[T % 2]
                K.dma("sp", x_[:], xb[T * 128:(T + 1) * 128, :], writes=[xB])
                ss_, ssB = ssb[T % 4]
                rs_, rsB = rsb[T % 4]
                xs_, xsB = xs[T % 2]
                K.op("act", lambda e: e.activation(out=xs_[:], in_=x_[:], func=AF.Square, accum_out=ss_[:]),
                     reads=[xB], writes=[ssB, xsB])
                K.op("act", lambda e: e.activation(out=rs_[:], in_=ss_[:], func=AF.Ln, bias=epsr[:, 0:1], scale=1.0 / D),
                     reads=[ssB], writes=[rsB])
                K.op("act", lambda e: e.activation(out=rs_[:], in_=rs_[:], func=AF.Exp, scale=-0.5), reads=[rsB], writes=[rsB])
                yield
                xs_, xsB = xs[T % 2]
                K.op("dve", lambda e: e.tensor_scalar(out=xs_[:], in0=x_[:], scalar1=rs_[:, 0:1], scalar2=None, op0=ALU.mult),
                     reads=[xB, rsB], writes=[xsB])
                yield
                tr, trB = TR.next()
                for c in range(8):
                    K.op("pe", lambda e, c=c: e.transpose(out=tr[:, c * 128:(c + 1) * 128], in_=xs_[:, c * 128:(c + 1) * 128],
                                                          identity=idb[:]), reads=[xsB], writes=[trB])
                copy_op("dve", h[:, :, t * 128:(t + 1) * 128], tr[:].rearrange("p (c j) -> p c j", j=128),
                        reads=[trB], writes=[hB])
                yield

        def proj(g, js=(0, 1, 2, 3), do_tm=True):
            h, hB = hT[g % 2]
            for j in js:
                bk, bkB = fpb[0]
                for c in range(8):
                    K.op("pe", lambda e, c=c: e.matmul(bk[:], lhsT=W1b[:, c, j * 128:(j + 1) * 128], rhs=h[:, c, :],
                                                       start=(c == 0), stop=(c == 7)), reads=[hB], writes=[bkB])
                yield
                if j == 0:
                    for X in range(2):
                        q_, qB = QA[X][g % 2]
                        copy_op("act" if X == 0 else "dve", q_[0:64, :], bk[64 * X:64 * X + 64, :], [bkB], [qB], scale=0.125)
                elif j == 1:
                    for X in range(2):
                        copy_op("act" if X == 0 else "dve", KA[X][0:64, g * 512:(g + 1) * 512], bk[64 * X:64 * X + 64, :],
                                [bkB], [KAB[X][g]])
                    km_, kmsB = kms[g % 2]
                    K.op("dve", lambda e: e.tensor_reduce(out=km_[:], in_=bk[:].rearrange("p (a b) -> p a b", b=256),
                                                          axis=AX.X, op=ALU.add), reads=[bkB], writes=[kmsB])
                    for X in range(2):
                        K.op("dve", lambda e, X=X: e.tensor_scalar(out=kmT[X][0:64, 2 * g:2 * g + 2], in0=km_[64 * X:64 * X + 64, :],
                                                                   scalar1=1.0 / 256, scalar2=None, op0=ALU.mult),
                             reads=[kmsB], writes=[kmB_[X]])
                elif j == 2:
                    copy_op("act", rqT[g % 2][0][:], bk[:], [bkB], [rqT[g % 2][1]])
                    K.op("dve", lambda e: e.tensor_tensor(out=rqTd[g % 2][0][:], in0=bk[:], in1=qdec[:], op=ALU.mult),
                         reads=[bkB], writes=[rqTd[g % 2][1]])
                else:
                    copy_op("act", rkT[g % 2][0][:], bk[:], [bkB], [rkT[g % 2][1]], scale=128 ** -0.5)
                yield
            for t in (range(4) if do_tm else ()):
                bk, bkB = fpb[0]
                kc = 4 * g + t
                for c in range(8):
                    K.op("pe", lambda e, c=c: e.matmul(bk[:], lhsT=h[:, c, t * 128:(t + 1) * 128], rhs=W1b[:, c, 512:1024],
                                                       start=(c == 0), stop=(c == 7)), reads=[hB], writes=[bkB])
                yield
                copy_op("act", VV[:, kc, 0:64], bk[:, 0:64], [bkB], [VAB[g]])
                copy_op("dve", VV[:, kc, 128:192], bk[:, 64:128], [bkB], [VAB[g]])
                K.op("dve", lambda e: e.tensor_scalar(out=rkd[g % 2][0][:, t, :], in0=bk[:, 128:256],
                                                      scalar1=kdec[:, t % 2:t % 2 + 1], scalar2=None, op0=ALU.mult),
                     reads=[bkB], writes=[rkd[g % 2][1]])
                copy_op("act", rvt[g % 2][0][:, t, :], bk[:, 256:384], [bkB], [rvt[g % 2][1]])
                K.op("act", lambda e: e.activation(out=sg[g % 2][0][:, t, :], in_=bk[:, 384:512], func=AF.Exp, scale=-1.0),
                     reads=[bkB], writes=[sg[g % 2][1]])
                K.op("dve", lambda e: e.tensor_scalar_add(out=sg[g % 2][0][:, t, :], in0=sg[g % 2][0][:, t, :], scalar1=1.0),
                     reads=[sg[g % 2][1]], writes=[sg[g % 2][1]])
                K.op("dve", lambda e: e.reciprocal(out=sg[g % 2][0][:, t, :], in_=sg[g % 2][0][:, t, :]),
                     reads=[sg[g % 2][1]], writes=[sg[g % 2][1]])
                K.op("dve", lambda e: e.tensor_tensor(out=sg[g % 2][0][:, t, :], in0=sg[g % 2][0][:, t, :], in1=bk[:, 384:512], op=ALU.mult),
                     reads=[bkB, sg[g % 2][1]], writes=[sg[g % 2][1]])
                K.op("dve", lambda e: e.tensor_tensor(out=sg[g % 2][0][:, t, :], in0=sg[g % 2][0][:, t, :], in1=rng_[:],
                                                      op=ALU.mult), reads=[sg[g % 2][1]], writes=[sg[g % 2][1]])
                yield

        MI = [0]

        def masks(g):
            bk, bkB = fpb[0]
            for t in range(4):
                for X in range(2):
                    q_, qB = QA[X][g % 2]
                    o = (4 * X + t) * 64
                    K.op("pe", lambda e: e.matmul(bk[:, o:o + 64], lhsT=q_[0:64, t * 128:(t + 1) * 128], rhs=kmT[X][0:64, 0:64],
                                                  start=True, stop=True), reads=[qB, kmB_[X]], writes=[bkB])
            yield
            mbis = []
            for t in range(4):
                a = 2 * g + t // 2
                par = t % 2
                for X in range(2):
                    o = (4 * X + t) * 64
                    i = MI[0] % 2
                    MI[0] += 1
                    K.op("dve", lambda e: e.tensor_tensor(out=Gm[i][0][:], in0=bk[:, o:o + 64], in1=Ctab[:, 64 - a:128 - a], op=ALU.add),
                         reads=[bkB], writes=[Gm[i][1]])
                    K.op("dve", lambda e: e.max(out=m8[i][0][:], in_=Gm[i][0][:]), reads=[Gm[i][1]], writes=[m8[i][1]])
                    K.op("dve", lambda e: e.tensor_scalar_max(out=thr[i][0][:], in0=m8[i][0][:, 2:3], scalar1=-1e29),
                         reads=[m8[i][1]], writes=[thr[i][1]])
                    K.op("dve", lambda e: e.tensor_scalar(out=mb32[i][0][:], in0=Gm[i][0][:], scalar1=thr[i][0][:, 0:1],
                                                          scalar2=BIG, op0=ALU.is_ge, op1=ALU.mult),
                         reads=[Gm[i][1], thr[i][1]], writes=[mb32[i][1]])
                    mbi, mbiB = mbin[X][par][(t // 2) % 2]
                    K.op("dve", lambda e: e.tensor_tensor(out=mbi[:, 64:127], in0=mb32[i][0][:, 0:63],
                                                          in1=dtab[:, 128 * X + 64 - a:128 * X + 127 - a], op=ALU.add),
                         reads=[mb32[i][1]], writes=[mbiB])
                    mbis.append((t, X, mbi, mbiB))
                    yield
            tr, trB = TR.next()
            for (t, X, mbi, mbiB) in mbis:
                o = (4 * X + t) * 128
                K.op("pe", lambda e: e.transpose(out=tr[:, o:o + 128], in_=mbi[:], identity=idb[:]), reads=[mbiB], writes=[trB])
            for X in range(2):
                q_, qB = QA[X][g % 2]
                copy_op("act" if X == 0 else "dve", q_[64:128, :], tr[64:128, X * 512:(X + 1) * 512], [trB], [qB])
            yield

        def attn(g, side):
            items = []
            firsts = [[True], [True]]

            def mk_pv(X, kc, lo, hi, ptref, last=False):
                ob, obB = obk[X]
                vlo = 64 * X

                def f():
                    pt, ptB = ptref[0]
                    K.op("pe", lambda e: e.matmul(ob[:, lo:hi], lhsT=VV[:, kc, vlo:vlo + 128], rhs=pt[:, lo:hi],
                                                  start=firsts[X][0], stop=last), reads=[ptB, VAB[kc // 4]], writes=[obB])
                    firsts[X][0] = False
                return f

            def past_item(X, kc, c):
                q_, qB = QA[X][g % 2]
                sref, ptref = [None], [None]

                def qk():
                    sref[0] = AFP.next()
                    sbk, sbB = sref[0]
                    K.op("pe", lambda e: e.matmul(sbk[:], lhsT=KA[X][:, kc * 128:(kc + 1) * 128], rhs=q_[:, :], start=True, stop=True),
                         reads=[qB, KAB[X][kc // 4]], writes=[sbB])

                def ex():
                    sbk, sbB = sref[0]
                    ptref[0] = PT.next()
                    pt, ptB = ptref[0]
                    K.op("act", lambda e: e.activation(out=pt[:], in_=sbk[:], func=AF.Exp, bias=ekb[:, 2 * X + c:2 * X + c + 1], scale=1.0),
                         reads=[sbB], writes=[ptB])
                return (qk, ex, mk_pv(X, kc, 0, 512, ptref))

            def diag0_item(X, c):
                q_, qB = QA[X][g % 2]
                kc = 4 * g + c
                q0 = 0 if c == 0 else 128
                sref, ptref = [None], [None]

                def qk():
                    sref[0] = AFP.next()
                    sbk, sbB = sref[0]
                    K.op("pe", lambda e: e.matmul(sbk[:, q0:256], lhsT=KA[X][0:64, kc * 128:(kc + 1) * 128], rhs=q_[0:64, q0:256],
                                                  start=True, stop=False), reads=[qB, KAB[X][kc // 4]], writes=[sbB])
                    K.op("pe", lambda e: e.matmul(sbk[:, q0:256], lhsT=idb[:], rhs=bdiag[:, X, c, q0:256], start=False, stop=True),
                         writes=[sbB])
                    K.op("pe", lambda e: e.matmul(sbk[:, 256:512], lhsT=KA[X][:, kc * 128:(kc + 1) * 128], rhs=q_[:, 256:512],
                                                  start=True, stop=True), reads=[qB, KAB[X][kc // 4]], writes=[sbB])

                def ex():
                    sbk, sbB = sref[0]
                    ptref[0] = PT.next()
                    pt, ptB = ptref[0]
                    K.op("act", lambda e: e.activation(out=pt[:, q0:256], in_=sbk[:, q0:256], func=AF.Exp), reads=[sbB], writes=[ptB])
                    K.op("act", lambda e: e.activation(out=pt[:, 256:512], in_=sbk[:, 256:512], func=AF.Exp,
                                                       bias=ekb[:, 2 * X + c:2 * X + c + 1], scale=1.0), reads=[sbB], writes=[ptB])
                return (qk, ex, mk_pv(X, kc, q0, 512, ptref))

            def diag1_item(X, c):
                q_, qB = QA[X][g % 2]
                kc = 4 * g + 2 + c
                q0 = 256 if c == 0 else 384
                sref, ptref = [None], [None]

                def qk():
                    sref[0] = AFP.next()
                    sbk, sbB = sref[0]
                    K.op("pe", lambda e: e.matmul(sbk[:, q0:512], lhsT=KA[X][0:64, kc * 128:(kc + 1) * 128], rhs=q_[0:64, q0:512],
                                                  start=True, stop=False), reads=[qB, KAB[X][kc // 4]], writes=[sbB])
                    K.op("pe", lambda e: e.matmul(sbk[:, q0:512], lhsT=idb[:], rhs=bdiag[:, X, c, q0 - 256:256], start=False, stop=True),
                         writes=[sbB])

                def ex():
                    sbk, sbB = sref[0]
                    ptref[0] = PT.next()
                    pt, ptB = ptref[0]
                    K.op("act", lambda e: e.activation(out=pt[:, q0:512], in_=sbk[:, q0:512], func=AF.Exp), reads=[sbB], writes=[ptB])
                return (qk, ex, mk_pv(X, kc, q0, 512, ptref, last=(c == 1)))

            if g == 0:
                for c in range(2):
                    for X in range(2):
                        items.append(diag0_item(X, c))
            for n in range(2 * g):
                for c in range(2):
                    for X in range(2):
                        if X == 0 and n < 2 * g - WIN0:
                            continue
                        items.append(past_item(X, 2 * n + c, c))
            if g > 0:
                for c in range(2):
                    for X in range(2):
                        items.append(diag0_item(X, c))
            for c in range(2):
                for X in range(2):
                    items.append(diag1_item(X, c))
            LA = 2
            stride = max(1, len(items) // 80)
            burst = max(1, -(-80 // len(items)))
            for i in range(len(items) + LA):
                if i < len(items):
                    items[i][0]()
                j = i - LA
                if j >= 0:
                    items[j][1]()
                    items[j][2]()
                if i % stride == 0:
                    for _ in range(burst):
                        next(side, None)
            for _ in side:
                pass
            for X in range(2):
                ob, obB = obk[X]
                r_, rB = rc[X]
                m_, mB = mixst[g % 2]
                nlo, dlo = (0, 64) if X == 0 else (64, 0)
                K.op("dve", lambda e: e.reciprocal(out=r_[nlo:nlo + 64, :], in_=ob[dlo:dlo + 64, :]), reads=[obB], writes=[rB])
                K.op("dve", lambda e: e.tensor_tensor(out=m_[nlo:nlo + 64, :], in0=ob[nlo:nlo + 64, :], in1=r_[nlo:nlo + 64, :], op=ALU.mult),
                     reads=[obB, rB], writes=[mB])

        CUR = [cur_st]

        def ret(g):
            i2 = g % 2
            rq_, rqB = rqT[i2]
            rqd_, rqdB = rqTd[i2]
            rk_, rkB = rkT[i2]
            rkd_, rkdB = rkd[i2]
            rv_, rvB = rvt[i2]
            gs_, gsB = gs[i2]
            rst, rstB = retst[i2]
            for ch in range(2):
                c0 = 256 * ch
                at, atB = AT.next()
                sbk, sbB = fpb[1]
                ub, ubB = fpb[1]
                K.op("pe", lambda e: e.matmul(sbk[:, 0:256], lhsT=rk_[:, c0:c0 + 128], rhs=rq_[:, c0:c0 + 256],
                                              start=True, stop=True), reads=[rkB, rqB], writes=[sbB])
                K.op("pe", lambda e: e.matmul(sbk[:, 256:384], lhsT=rk_[:, c0 + 128:c0 + 256], rhs=rq_[:, c0 + 128:c0 + 256],
                                              start=True, stop=True), reads=[rkB, rqB], writes=[sbB])
                for kh in range(2):
                    K.op("pe", lambda e: e.matmul(ub[:, 384:512], lhsT=rkd_[:, 2 * ch + kh, :], rhs=rv_[:, 2 * ch + kh, :],
                                                  start=(kh == 0), stop=(kh == 1)), reads=[rkdB, rvB], writes=[ubB])
                yield
                K.op("dve", lambda e: e.tensor_tensor(out=at[:, 0, 0:256], in0=sbk[:, 0:256], in1=decT[:, 0, 0:256], op=ALU.mult),
                     reads=[sbB], writes=[atB])
                K.op("dve", lambda e: e.tensor_tensor(out=at[:, 1, 128:256], in0=sbk[:, 256:384], in1=decT[:, 1, 128:256], op=ALU.mult),
                     reads=[sbB], writes=[atB])
                st_, stB_ = CUR[0]
                K.op("dve", lambda e: e.scalar_tensor_tensor(out=state[:], in0=state[:], scalar=cdc[:, 0:1], in1=ub[:, 384:512],
                                                             op0=ALU.mult, op1=ALU.add), reads=[ubB, stateB], writes=[stateB])
                nst = stbf.next()
                K.op("dve", lambda e: e.tensor_copy(out=nst[0][:], in_=state[:]), reads=[stateB], writes=[nst[1]])
                CUR[0] = nst
                yield
                rb, rbB = fpb[1]
                for qt in range(2):
                    o = 128 * qt
                    K.op("pe", lambda e: e.matmul(rb[:, o:o + 128], lhsT=at[:, 0, qt * 128:(qt + 1) * 128], rhs=rv_[:, 2 * ch, :],
                                                  start=True, stop=False), reads=[atB, rvB], writes=[rbB])
                    if qt == 1:
                        K.op("pe", lambda e: e.matmul(rb[:, o:o + 128], lhsT=at[:, 1, 128:256], rhs=rv_[:, 2 * ch + 1, :],
                                                      start=False, stop=False), reads=[atB, rvB], writes=[rbB])
                    K.op("pe", lambda e: e.matmul(rb[:, o:o + 128], lhsT=rqd_[:, c0 + qt * 128:c0 + qt * 128 + 128], rhs=st_[:],
                                                  start=False, stop=True), reads=[rqdB, stB_], writes=[rbB])
                yield
                ybs = []
                for qt in range(2):
                    o = 128 * qt
                    s6, s6B = st6.next()
                    mv_, mvB = mv.next()
                    r2, r2B = rstd2.next()
                    y_, yB = yn.next()
                    yb_, ybB = ynb.next()
                    K.op("dve", lambda e: e.bn_stats(out=s6[:], in_=rb[:, o:o + 128]), reads=[rbB], writes=[s6B])
                    K.op("dve", lambda e: e.bn_aggr(out=mv_[:], in_=s6[:]), reads=[s6B], writes=[mvB])
                    K.op("act", lambda e: e.activation(out=r2[:], in_=mv_[:, 1:2], func=AF.Ln, bias=epsg[:, 0:1], scale=1.0),
                         reads=[mvB], writes=[r2B])
                    K.op("act", lambda e: e.activation(out=r2[:], in_=r2[:], func=AF.Exp, scale=-0.5), reads=[r2B], writes=[r2B])
                    yield
                    K.op("dve", lambda e: e.tensor_scalar(out=y_[:], in0=rb[:, o:o + 128], scalar1=mv_[:, 0:1], scalar2=r2[:, 0:1],
                                                          op0=ALU.subtract, op1=ALU.mult), reads=[rbB, mvB, r2B], writes=[yB])
                    K.op("dve", lambda e: e.tensor_tensor(out=yb_[:], in0=y_[:], in1=gs_[:, 2 * ch + qt, :], op=ALU.mult),
                         reads=[yB, gsB], writes=[ybB])
                    ybs.append((yb_, ybB))
                    yield
                tr, trB = TR.next()
                for qt in range(2):
                    K.op("pe", lambda e: e.transpose(out=tr[:, qt * 128:(qt + 1) * 128], in_=ybs[qt][0][:], identity=idb[:]),
                         reads=[ybs[qt][1]], writes=[trB])
                copy_op("dve", rst[:, c0:c0 + 256], tr[:, 0:256], [trB], [rstB])
                yield

        agiB = [Buf("agi%d" % k) for k in range(NSEG)]
        agsem = [K.newsem("ag") for _ in range(NSEG)]
        mineB = Buf("mine")

        def outp(g):
            k = g // 4
            c0 = (g % 4) * 512
            m_, mB = mixst[g % 2]
            r_, rB = retst[g % 2]
            K.dma("sp", agi[k].ap()[0:128, c0:c0 + 512], m_[:], reads=[mB], writes=[agiB[k]])
            K.dma("sp", agi[k].ap()[128:256, c0:c0 + 512], r_[:], reads=[rB], writes=[agiB[k]])
            if "mix" in dbg:
                K.dma("sp", dbg["mix"][0:128, g * 512:(g + 1) * 512], m_[:], reads=[mB])
                K.dma("sp", dbg["mix"][128:256, g * 512:(g + 1) * 512], r_[:], reads=[rB])
            if g % 4 == 3:
                K.deps("pool", [agiB[k]], [])
                nc.gpsimd.collective_compute(
                    "AllGather", ALU.bypass, replica_groups=[[0, 1, 2, 3], [4, 5, 6, 7]],
                    ins=[agi[k].ap().opt()], outs=[ago[k].ap().opt()],
                ).then_inc(agsem[k].h)
                agsem[k].v = 1

        QV = [None]

        def agcopy(k):
            nc.gpsimd.wait_ge(agsem[k].h, 1)
            if QV[0] is None:
                QV[0] = nc.gpsimd.snap(nc.gpsimd.partition_id() % 4)
            qv = QV[0]
            slot = (qv * 6 + k) % 8
            K.dma("pool", mine.ap()[:, bass.ds(slot * SEGT, SEGT)], ago[k].ap()[:, :], writes=[mineB])

        import itertools
        PW = 512
        wst_f = [(s1("wcf%d" % i, [128, PW]), Buf("wcf%d" % i)) for i in range(2)]
        wst_b = [(s1("wcb%d" % i, [128, PW], BF16), Buf("wcb%d" % i)) for i in range(2)]
        WCN = [0]

        def wpiece(src, dst, dst3, gain, n=PW):
            i = WCN[0] % 2
            WCN[0] += 1
            (st, stB), (sbf, sbB) = wst_f[i], wst_b[i]
            K.dma("sp", st[:, 0:n], src, writes=[stB])
            if gain is None:
                K.op("pool", lambda e: e.tensor_copy(out=sbf[:, 0:n], in_=st[:, 0:n]), reads=[stB], writes=[sbB])
            else:
                K.op("pool", lambda e: e.tensor_scalar(out=sbf[:, 0:n], in0=st[:, 0:n], scalar1=gain, scalar2=None, op0=ALU.mult),
                     reads=[stB], writes=[sbB])
            K.dma("pool", dst, sbf[:, 0:n].rearrange("p (f j) -> p f j", j=128) if dst3 else sbf[:, 0:n], reads=[sbB])

        def wconv():
            for c in range(8):
                rows = slice(c * 128, (c + 1) * 128)
                for wi, wsrc in enumerate((wup, wgate)):
                    for fo in range(0, NF, 4):
                        nf = min(4, NF - fo)
                        dst = wupg_s.ap()[fo:fo + nf, wi, :, c * 128:(c + 1) * 128].rearrange("f p j -> p f j")
                        wpiece(wsrc[rows, fo * 128:(fo + nf) * 128], dst, True, gains[:, c:c + 1], n=nf * 128)
                        yield
                for k4 in range(D // PW):
                    wpiece(wout[rows, k4 * PW:(k4 + 1) * PW], wout_s.ap()[:, c * D + k4 * PW:c * D + (k4 + 1) * PW], False, None)
                    yield
                    wpiece(wpg[rows, k4 * PW:(k4 + 1) * PW], wpg_s.ap()[:, c * D + k4 * PW:c * D + (k4 + 1) * PW], False,
                           gains[:, 8 + c:9 + c])
                    yield
            for f in range(NF):
                for k4 in range(D // PW):
                    wpiece(wdown[f * 128:(f + 1) * 128, k4 * PW:(k4 + 1) * PW],
                           wdown_s.ap()[:, f * D + k4 * PW:f * D + (k4 + 1) * PW], False, None)
                    yield
            for c in range(2):
                for k4 in range(D // PW):
                    wpiece(wple[c * 128:(c + 1) * 128, k4 * PW:(k4 + 1) * PW],
                           wple_s.ap()[:, c * D + k4 * PW:c * D + (k4 + 1) * PW], False, None)
                    yield
        WG = wconv()
        NPIECE = 8 * (2 * 6 + 4) + NF * 2 + 4
        PER_G = -(-NPIECE // NG)

        def run(gen):
            for _ in gen:
                pass

        def roundrobin(gens):
            live = list(gens)
            while live:
                for g_ in list(live):
                    if next(g_, "end") == "end":
                        live.remove(g_)
                    else:
                        yield
        run(prep(0))
        run(proj(0))
        run(masks(0))
        run(prep(1))
        for g in range(NG):
            side = [ret(g)]
            if g + 1 < NG:
                side += [itertools.chain(proj(g + 1, (0, 1), False), masks(g + 1), proj(g + 1, (2, 3), True))]
            if g + 2 < NG:
                side += [prep(g + 2)]
            side += [itertools.islice(WG, PER_G)]
            attn(g, roundrobin(side))
            outp(g)
            if g % 4 == 1 and g >= 5:
                agcopy((g - 5) // 4)
        run(WG)
        agcopy(NSEG - 1)
        K.barrier()

    with contextlib.ExitStack() as p2:
        def s2(name, shape, dt=F32):
            return p2.enter_context(nc.sbuf_tensor(name, list(shape), dt))

        wdn = s2("wdn", [128, NF, D], BF16)
        wo = s2("wo", [128, 8, D], BF16)
        wg = s2("wg", [128, 8, D], BF16)
        wp = s2("wp", [128, 2, D], BF16)
        cws = s2("cws", [128, NF, 3])
        cbs = s2("cbs", [128, NF])
        fins = s2("fins", [128, D])
        hfl = s2("hfl", [128, 1])
        cB = Buf("c2")
        K.dma("sp", wdn[:].rearrange("p f n -> p (f n)"), wdown_s.ap()[:, :], writes=[cB])
        K.dma("sp", wo[:].rearrange("p f n -> p (f n)"), wout_s.ap()[:, :], writes=[cB])
        K.dma("sp", wg[:].rearrange("p f n -> p (f n)"), wpg_s.ap()[:, :], writes=[cB])
        K.dma("sp", wp[:].rearrange("p f n -> p (f n)"), wple_s.ap()[:, :], writes=[cB])
        K.dma("sp", cws[:].rearrange("p f n -> p (f n)"), cw[:, :], writes=[cB])
        K.dma("sp", cbs[:], cb[:, :], writes=[cB])
        K.dma("sp", fins[:], fin[:, :], writes=[cB])
        K.dma("sp", hfl[:], hflag[:, :], writes=[cB])

        mixT = [(s2("mixT%d" % i, [128, 8, 512], BF16), Buf("mixT%d" % i)) for i in range(2)]
        xr = s2("xr", [128, 4, D])
        xrB = [Buf("xr%d" % t) for t in range(4)]
        pin = [(s2("pin%d" % i, [128, 256]), Buf("pin%d" % i)) for i in range(2)]
        pbf = [(s2("pbf%d" % i, [128, 256], BF16), Buf("pbf")) for i in range(2)]
        pT = s2("pT", [128, 2, 512], BF16)
        pTB = Buf("pT")
        junk2 = s2("junk2", [128, D], BF16)
        ss2 = Rot([(s2("ss2%d" % i, [128, 1]), Buf("ss2")) for i in range(4)])
        xs2 = Rot([(s2("xs2%d" % i, [128, D], BF16), Buf("xs2")) for i in range(2)])
        hT2 = s2("hT2", [128, 8, 512], BF16)
        hT2B = Buf("hT2")
        actT = s2("actT", [128, NF, 512], BF16)
        actB = [Buf("act%d" % f) for f in range(NF)]
        uh = s2("uh", [128, NF, 2])
        uhB = [Buf("uh%d" % f) for f in range(NF)]
        usb = Rot([(s2("usb%d" % i, [128, 514]), Buf("usb")) for i in range(2)])
        yc = Rot([(s2("yc%d" % i, [128, 512]), Buf("yc")) for i in range(2)])
        gl = Rot([(s2("gl%d" % i, [128, 512]), Buf("gl")) for i in range(2)])
        WS = Rot([(s2("ws%d" % i, [128, 2, 8 * 128], BF16), Buf("ws%d" % i)) for i in range(3)])
        sgm = Rot([(s2("sgm%d" % i, [128, 512]), Buf("sgm")) for i in range(2)])
        yo = Rot([(s2("yo%d" % i, [128, D]), Buf("yo%d" % i)) for i in range(2)])
        xhs = s2("xhs", [2, D])
        xhsB = Buf("xhs")
        mixh = s2("mixh", [128, 8, 2], BF16)
        mixhB = Buf("mixh")
        hhT = s2("hhT", [128, 8, 2], BF16)
        hhTB = Buf("hhT")
        xsh = s2("xsh", [2, D], BF16)
        xshB = Buf("xsh")
        ssh = s2("ssh", [2, 1])
        sshB = Buf("ssh")
        for f in range(NF):
            pass
        K.barrier()

        def rms_to_hT(t, xin_ap, xin_B, dst, dstB, npart=128):
            ss_, ssB = ss2.next()
            xs_, xsB = xs2.next()
            K.op("act", lambda e: e.activation(out=junk2[0:npart, :], in_=xin_ap, func=AF.Square, accum_out=ss_[0:npart, :]),
                 reads=[xin_B], writes=[ssB])
            K.op("act", lambda e: e.activation(out=ss_[0:npart, :], in_=ss_[0:npart, :], func=AF.Ln, bias=epsr[0:npart, 0:1], scale=1.0 / D),
                 reads=[ssB], writes=[ssB])
            K.op("act", lambda e: e.activation(out=ss_[0:npart, :], in_=ss_[0:npart, :], func=AF.Exp, scale=-0.5), reads=[ssB], writes=[ssB])
            K.op("dve", lambda e: e.tensor_scalar(out=xs_[0:npart, :], in0=xin_ap, scalar1=ss_[0:npart, 0:1], scalar2=None, op0=ALU.mult),
                 reads=[xin_B, ssB], writes=[xsB])
            tr, trB = TR.next()
            for c in range(8):
                K.op("pe", lambda e, c=c: e.transpose(out=tr[:, c * 128:c * 128 + npart], in_=xs_[0:npart, c * 128:(c + 1) * 128],
                                                      identity=idb[0:npart, 0:npart]), reads=[xsB], writes=[trB])
            if npart == 128:
                copy_op(evac_eng(), dst[:, :, t * 128:(t + 1) * 128], tr[:].rearrange("p (c j) -> p c j", j=128), [trB], [dstB])
            else:
                copy_op("dve", dst[:, :, 0:npart], tr[:].rearrange("p (c j) -> p c j", j=128)[:, :, 0:npart], [trB], [dstB])
            return ss_, ssB

        def load_mix(j, buf):
            m_, mB = buf
            seg = j // 4
            c0 = (j % 4) * 512
            K.dma("sp", m_[:], mine.ap()[:, seg * SEGT + c0:seg * SEGT + c0 + 512].rearrange("(c p) t -> p c t", p=128), reads=[mineB], writes=[mB])

        K.dma("sp", xhs[:], xh[:, :], writes=[xhsB])
        K.dma("sp", mixh[:], mine.ap()[:, 8 * SEGT - 2:8 * SEGT].rearrange("(c p) t -> p c t", p=128), reads=[mineB], writes=[mixhB])
        for hf in range(2):
            bk, bkB = FP2.next()
            for c in range(8):
                K.op("pe", lambda e, c=c: e.matmul(bk[0:2, :], lhsT=mixh[:, c, :], rhs=wo[:, c, hf * 512:(hf + 1) * 512],
                                                   start=(c == 0), stop=(c == 7)), reads=[mixhB], writes=[bkB])
            K.op("dve", lambda e: e.tensor_tensor(out=xhs[:, hf * 512:(hf + 1) * 512], in0=bk[0:2, :], in1=xhs[:, hf * 512:(hf + 1) * 512],
                                                  op=ALU.add), reads=[bkB, xhsB], writes=[xhsB])
        rms_to_hT(0, xhs[:], xhsB, hhT, hhTB, npart=2)

        load_mix(0, mixT[0])
        for j in range(NT2):
            m_, mB = mixT[j % 2]
            if j + 1 < NT2:
                load_mix(j + 1, mixT[(j + 1) % 2])
            for t in range(4):
                r0 = j * 512 + t * 128
                K.dma("sp", xr[:, t, :], xq[r0:r0 + 128, :], writes=[xrB[t]])
                p_, pB = pin[t % 2]
                K.dma("sp", p_[:], pq[r0:r0 + 128, :], writes=[pB])
                pb_, pbB = pbf[t % 2]
                K.op("pool", lambda e: e.tensor_copy(out=pb_[:], in_=p_[:]), reads=[pB], writes=[pbB])
                tr, trB = TR.next()
                for c in range(2):
                    K.op("pe", lambda e, c=c: e.transpose(out=tr[:, c * 128:(c + 1) * 128], in_=pb_[:, c * 128:(c + 1) * 128],
                                                          identity=idb[:]), reads=[pbB], writes=[trB])
                copy_op(evac_eng(), pT[:, :, t * 128:(t + 1) * 128], tr[:, 0:256].rearrange("p (c j) -> p c j", j=128), [trB], [pTB])
                for hf in range(2):
                    bk, bkB = FP2.next()
                    for c in range(8):
                        K.op("pe", lambda e, c=c: e.matmul(bk[:], lhsT=m_[:, c, t * 128:(t + 1) * 128], rhs=wo[:, c, hf * 512:(hf + 1) * 512],
                                                           start=(c == 0), stop=(c == 7)), reads=[mB], writes=[bkB])
                    K.op("dve", lambda e: e.tensor_tensor(out=xr[:, t, hf * 512:(hf + 1) * 512], in0=bk[:], in1=xr[:, t, hf * 512:(hf + 1) * 512],
                                                          op=ALU.add), reads=[bkB, xrB[t]], writes=[xrB[t]])
                rms_to_hT(t, xr[:, t, :], xrB[t], hT2, hT2B)
            for f in range(NF):
                w_, wB = WS.next()
                K.dma("sp", w_[:], wupg_s.ap()[f].rearrange("a p n -> p a n"), writes=[wB])
                ub, ubB = FP2.next()
                gb, gbB = FP2.next()
                for c in range(8):
                    K.op("pe", lambda e, c=c: e.matmul(ub[:], lhsT=w_[:, 0, c * 128:(c + 1) * 128], rhs=hT2[:, c, :],
                                                       start=(c == 0), stop=(c == 7)), reads=[wB, hT2B], writes=[ubB])
                for c in range(8):
                    K.op("pe", lambda e, c=c: e.matmul(gb[:], lhsT=w_[:, 1, c * 128:(c + 1) * 128], rhs=hT2[:, c, :],
                                                       start=(c == 0), stop=(c == 7)), reads=[wB, hT2B], writes=[gbB])
                u_, uB = usb.next()
                if j == 0:
                    hb, hbB = FP2.next()
                    for c in range(8):
                        K.op("pe", lambda e, c=c: e.matmul(hb[:, 0:2], lhsT=w_[:, 0, c * 128:(c + 1) * 128], rhs=hhT[:, c, :],
                                                           start=(c == 0), stop=(c == 7)), reads=[wB, hhTB], writes=[hbB])
                    K.op("dve", lambda e: e.tensor_scalar(out=u_[:, 0:2], in0=hb[:, 0:2], scalar1=hfl[:, 0:1], scalar2=None, op0=ALU.mult),
                         reads=[hbB], writes=[uB])
                else:
                    K.op("pool", lambda e, f=f: e.tensor_copy(out=u_[:, 0:2], in_=uh[:, f, :]), reads=[uhB[f]], writes=[uB])
                copy_op("act", u_[:, 2:514], ub[:], [ubB], [uB])
                K.op("pool", lambda e, f=f: e.tensor_copy(out=uh[:, f, :], in_=u_[:, 512:514]), reads=[uB], writes=[uhB[f]])
                y_, yB = yc.next()
                K.op("dve", lambda e, f=f: e.tensor_scalar(out=y_[:], in0=u_[:, 2:514], scalar1=cws[:, f, 2:3], scalar2=cbs[:, f:f + 1],
                                                           op0=ALU.mult, op1=ALU.add), reads=[uB], writes=[yB])
                K.op("dve", lambda e, f=f: e.scalar_tensor_tensor(out=y_[:], in0=u_[:, 1:513], scalar=cws[:, f, 1:2], in1=y_[:],
                                                                  op0=ALU.mult, op1=ALU.add), reads=[uB, yB], writes=[yB])
                K.op("dve", lambda e, f=f: e.scalar_tensor_tensor(out=y_[:], in0=u_[:, 0:512], scalar=cws[:, f, 0:1], in1=y_[:],
                                                                  op0=ALU.mult, op1=ALU.add), reads=[uB, yB], writes=[yB])
                g_, gB_ = gl.next()
                K.op("act", lambda e: e.activation(out=g_[:], in_=y_[:], func=AF.Gelu_apprx_tanh), reads=[yB], writes=[gB_])
                K.op("dve", lambda e, f=f: e.tensor_tensor(out=actT[:, f, :], in0=g_[:], in1=gb[:], op=ALU.mult),
                     reads=[gB_, gbB], writes=[actB[f]])
            for t in range(4):
                for hf in range(2):
                    bk, bkB = FP2.next()
                    for f in range(NF):
                        K.op("pe", lambda e, f=f: e.matmul(bk[:], lhsT=actT[:, f, t * 128:(t + 1) * 128], rhs=wdn[:, f, hf * 512:(hf + 1) * 512],
                                                           start=(f == 0), stop=(f == NF - 1)), reads=[actB[f]], writes=[bkB])
                    K.op("dve", lambda e: e.tensor_tensor(out=xr[:, t, hf * 512:(hf + 1) * 512], in0=bk[:], in1=xr[:, t, hf * 512:(hf + 1) * 512],
                                                          op=ALU.add), reads=[bkB, xrB[t]], writes=[xrB[t]])
            for t in range(4):
                rms_to_hT(t, xr[:, t, :], xrB[t], hT2, hT2B)
            for t in range(4):
                for hf in range(2):
                    bk, bkB = FP2.next()
                    for c in range(8):
                        K.op("pe", lambda e, c=c: e.matmul(bk[:], lhsT=hT2[:, c, t * 128:(t + 1) * 128], rhs=wg[:, c, hf * 512:(hf + 1) * 512],
                                                           start=(c == 0), stop=(c == 7)), reads=[hT2B], writes=[bkB])
                    s_, sB = sgm.next()
                    K.op("act", lambda e: e.activation(out=s_[:], in_=bk[:], func=AF.Sigmoid), reads=[bkB], writes=[sB])
                    bk2, bk2B = FP2.next()
                    for c in range(2):
                        K.op("pe", lambda e, c=c: e.matmul(bk2[:], lhsT=pT[:, c, t * 128:(t + 1) * 128], rhs=wp[:, c, hf * 512:(hf + 1) * 512],
                                                           start=(c == 0), stop=(c == 1)), reads=[pTB], writes=[bk2B])
                    K.op("dve", lambda e: e.tensor_tensor(out=s_[:], in0=bk2[:], in1=s_[:], op=ALU.mult), reads=[bk2B, sB], writes=[sB])
                    K.op("dve", lambda e: e.tensor_tensor(out=xr[:, t, hf * 512:(hf + 1) * 512], in0=s_[:], in1=xr[:, t, hf * 512:(hf + 1) * 512],
                                                          op=ALU.add), reads=[sB, xrB[t]], writes=[xrB[t]])
                ss_, ssB = ss2.next()
                K.op("act", lambda e: e.activation(out=junk2[:], in_=xr[:, t, :], func=AF.Square, accum_out=ss_[:]),
                     reads=[xrB[t]], writes=[ssB])
                K.op("act", lambda e: e.activation(out=ss_[:], in_=ss_[:], func=AF.Ln, bias=epsr[:, 0:1], scale=1.0 / D),
                     reads=[ssB], writes=[ssB])
                K.op("act", lambda e: e.activation(out=ss_[:], in_=ss_[:], func=AF.Exp, scale=-0.5), reads=[ssB], writes=[ssB])
                o_, oB = yo.next()
                K.op("dve", lambda e: e.scalar_tensor_tensor(out=o_[:], in0=xr[:, t, :], scalar=ss_[:, 0:1], in1=fins[:],
                                                             op0=ALU.mult, op1=ALU.mult), reads=[xrB[t], ssB], writes=[oB])
                r0 = j * 512 + t * 128
                K.dma("act", y[r0:r0 + 128, :], o_[:], reads=[oB])
        K.barrier()
    es.close()
    return nc


def _consts(g):
    bf = ml_dtypes.bfloat16
    out = {}
    kst = np.zeros((64, S), np.float32)
    for n in range(63):
        kst[n, n * 256:(n + 1) * 256] = 1.0
    kst[63, :] = 1.0
    out["c_kst"] = kst.astype(bf)
    slopes = [2.0 ** (-8.0 * (h + 1) / 8) for h in (g, 4 + g)]
    j = np.arange(128, dtype=np.float64)
    p = np.arange(128, dtype=np.float64)
    dtab = np.zeros((128, 2, 128), np.float64)
    negr = np.zeros((128, 2, 2), np.float64)
    ekb = np.zeros((128, 2, 2), np.float64)
    bd = np.zeros((128, 2, 2, 256), np.float64)
    r = np.arange(256, dtype=np.float64)
    for X, sl in enumerate(slopes):
        dtab[:, X, :] = (-BIG - sl * 256.0 * (64 - j))[None, :]
        for par in range(2):
            negr[:, X, par] = -sl * (par * 128 + p)
            ekb[:, X, par] = sl * (par * 128 + p)
        for c in range(2):
            t = (c * 128 + p)[:, None]
            bd[:, X, c, :] = np.where(r[None, :] >= t, -sl * (r[None, :] - t), -BIG)
    out["c_dtab"] = dtab.reshape(128, 256).astype(np.float32)
    out["c_negr"] = negr.reshape(128, 4).astype(np.float32)
    out["c_ekb"] = ekb.reshape(128, 4).astype(np.float32)
    out["c_bdiag"] = bd.reshape(128, 1024).astype(np.float32).astype(bf)
    gamma = 1.0 - 2.0 ** (-5.0 - g)
    lg = np.log(np.float64(gamma))
    dec = np.zeros((128, 2, 256), np.float64)
    for kh in range(2):
        t = (kh * 128 + p)[:, None]
        d_ = r[None, :] - t
        dec[:, kh, :] = np.where(d_ >= 0, np.exp(lg * np.maximum(d_, 0.0)), 0.0)
    out["c_decT"] = dec.reshape(128, 512).astype(np.float32)
    qd = np.exp(lg * (r + 1.0))
    out["c_qdec"] = np.tile(np.tile(qd, 2)[None, :], (128, 1)).astype(np.float32)
    kd = np.zeros((128, 2), np.float64)
    for kh in range(2):
        kd[:, kh] = np.exp(lg * (255.0 - (kh * 128 + p))) * (128.0 ** -0.5)
    out["c_kdec"] = kd.astype(np.float32)
    out["c_cd"] = np.full((128, 1), np.exp(lg * 256.0), np.float32)
    out["c_idb"] = np.eye(128, dtype=np.float32).astype(bf)
    return out


def _prep_inputs(x, p, attn_norm, w_in, ret_norm, w_out, ffn_norm, w_up, w_gate, conv_w, conv_b, w_down,
                 ple_norm, w_ple, w_ple_gate, final_norm):
    f32 = np.float32
    x = np.asarray(x, f32)
    p = np.asarray(p, f32)
    w_in0 = np.asarray(w_in, f32)[0]

    def pc(v):
        return np.ascontiguousarray(np.asarray(v, f32).reshape(8, 128).T)
    shared = {
        "an": pc(attn_norm[0]), "fn": pc(ffn_norm[0]), "pn": pc(ple_norm[0]),
        "wup": np.ascontiguousarray(np.asarray(w_up, f32)[0]), "wgate": np.ascontiguousarray(np.asarray(w_gate, f32)[0]),
        "wdown": np.ascontiguousarray(np.asarray(w_down, f32)[0]), "wple": np.ascontiguousarray(np.asarray(w_ple, f32)[0]),
        "wpg": np.ascontiguousarray(np.asarray(w_ple_gate, f32)[0]),
        "cw": np.ascontiguousarray(np.asarray(conv_w, f32)[0].reshape(3, NF, 128).transpose(2, 1, 0).reshape(128, NF * 3)),
        "cb": np.ascontiguousarray(np.asarray(conv_b, f32)[0].reshape(NF, 128).T),
        "fin": np.ascontiguousarray(np.tile(np.asarray(final_norm, f32)[None, :], (128, 1))),
    }
    wo = np.asarray(w_out, f32)[0]
    perm = []
    for gg in range(4):
        perm += (list(range(64 * gg, 64 * gg + 64)) + list(range(64 * (4 + gg), 64 * (4 + gg) + 64))
                 + list(range(512 + 128 * gg, 512 + 128 * gg + 128)))
    shared["wout"] = np.ascontiguousarray(wo[perm, :])
    rnorm = np.asarray(ret_norm, f32)[0]
    in_maps = []
    for c in range(8):
        b, g = c // 4, c % 4
        q = g
        hd = list(range(64 * g, 64 * g + 64)) + list(range(64 * (4 + g), 64 * (4 + g) + 64))
        cols_f = (hd + [512 + i for i in hd]
                  + list(range(1536 + 128 * g, 1536 + 128 * g + 128)) + list(range(2048 + 128 * g, 2048 + 128 * g + 128)))
        cols_t = ([1024 + i for i in hd] + list(range(2048 + 128 * g, 2048 + 128 * g + 128))
                  + list(range(2560 + 128 * g, 2560 + 128 * g + 128)) + list(range(3072 + 128 * g, 3072 + 128 * g + 128)))
        m = dict(shared)
        m["xb"] = x[b]
        m["xq"] = np.ascontiguousarray(x[b, q * TQ:(q + 1) * TQ])
        m["pq"] = np.ascontiguousarray(p[0, b, q * TQ:(q + 1) * TQ])
        m["xh"] = np.ascontiguousarray(x[b, q * TQ - 2:q * TQ]) if q > 0 else np.zeros((2, D), f32)
        m["hflag"] = np.full((128, 1), 1.0 if q > 0 else 0.0, f32)
        m["w1"] = np.ascontiguousarray(w_in0[:, cols_f + cols_t])
        m["rn"] = np.ascontiguousarray(np.tile(rnorm[128 * g:128 * g + 128][None, :], (128, 1)))
        m.update(_consts(g))
        in_maps.append(m)
    return in_maps


_NC_CACHE = {}


def kernel(x, p, attn_norm, w_in, ret_norm, w_out, ffn_norm, w_up, w_gate, conv_w, conv_b, w_down,
           ple_norm, w_ple, w_ple_gate, final_norm):
    in_maps = _prep_inputs(x, p, attn_norm, w_in, ret_norm, w_out, ffn_norm, w_up, w_gate, conv_w, conv_b, w_down,
                           ple_norm, w_ple, w_ple_gate, final_norm)
    nc = build_program()
    res = run_bass_kernel_spmd(nc, in_maps, core_ids=list(range(8)))
    out = np.empty((2, S, D), np.float32)
    for c in range(8):
        b, q = c // 4, c % 4
        out[b, q * TQ:(q + 1) * TQ] = np.asarray(res.results[c]["y"], np.float32)
    if DEBUG:
        DEBUG["_res"] = res
    return out
```

```python
import contextlib
import numpy as np
import ml_dtypes
import concourse.bass as bass
import concourse.mybir as mybir
from concourse.bass_utils import run_bass_kernel_spmd

F32 = mybir.dt.float32
BF16 = mybir.dt.bfloat16
ALU = mybir.AluOpType
AF = mybir.ActivationFunctionType
AX = mybir.AxisListType

S = 16384
D = 1024
NB = 64
NG = 32
NSEG = 8
SEGT = 2048
TQ = 4096
NT2 = 8
FF = 2816
NF = 22
BIG = 32768.0
WIN0 = 5
RMS_EPS = 1e-6
GN_EPS = 1e-5

DEBUG = {}


class Sem:
    def __init__(self, nc, name):
        self.h = nc.alloc_semaphore(name)
        self.v = 0


class Buf:
    __slots__ = ("name", "w", "r", "excl", "dsem")

    def __init__(self, name, excl=False):
        self.name = name
        self.w = None
        self.r = []
        self.excl = excl
        self.dsem = None


class Ctx:
    def __init__(self, nc):
        self.nc = nc
        self.eng = {"pe": nc.tensor, "act": nc.scalar, "dve": nc.vector, "pool": nc.gpsimd, "sp": nc.sync}
        self.esem = {k: Sem(nc, "e_" + k) for k in self.eng}
        self.waited = {k: {} for k in self.eng}
        self.nsem = 0
        self.dma_toks = []

    def newsem(self, name):
        self.nsem += 1
        return Sem(self.nc, "%s_%d" % (name, self.nsem))

    def wait(self, e, tok):
        sem, val = tok[0], tok[1]
        cur = self.waited[e].get(id(sem), 0)
        if cur >= val:
            return
        self.eng[e].wait_ge(sem.h, val)
        self.waited[e][id(sem)] = val

    def deps(self, e, reads, writes):
        toks = []
        for b in reads:
            if b.w is not None:
                toks.append(b.w)
            if b.excl:
                toks += [t for t in b.r if t[2] != e]
        for b in writes:
            if b.w is not None and (b.w[2] != e):
                toks.append(b.w)
            elif b.w is not None and b.excl and e != "pe":
                toks.append(b.w)
            toks += [t for t in b.r if t[2] != e]
        for t in toks:
            self.wait(e, t)

    def mark(self, tok, reads, writes):
        for b in reads:
            b.r.append(tok)
        for b in writes:
            b.w = tok
            b.r = []

    def op(self, e, fn, reads=(), writes=()):
        self.deps(e, reads, writes)
        ins = fn(self.eng[e])
        s = self.esem[e]
        s.v += 1
        ins.then_inc(s.h, 1)
        tok = (s, s.v, e)
        self.mark(tok, reads, writes)
        return tok

    def dma(self, q, out, in_, reads=(), writes=(), sem=None):
        self.deps(q, reads, writes)
        if sem is None:
            owner = None
            for b in list(writes) + list(reads):
                owner = b
                break
            if owner.dsem is None:
                owner.dsem = self.newsem("d")
            sem = owner.dsem
        ins = self.eng[q].dma_start(out=out, in_=in_)
        sem.v += 16
        ins.then_inc(sem.h, 16)
        tok = (sem, sem.v, None)
        self.mark(tok, reads, writes)
        self.dma_toks.append(tok)
        return tok

    def barrier(self):
        for e in self.eng:
            for e2, s in self.esem.items():
                if e2 != e and s.v > 0:
                    self.wait(e, (s, s.v, e2))
            for t in self.dma_toks:
                self.wait(e, t)
        self.dma_toks = []


class Rot:
    def __init__(self, items):
        self.items = items
        self.i = 0

    def next(self):
        it = self.items[self.i % len(self.items)]
        self.i += 1
        return it


def build_program():
    nc = bass.Bass("TRN2", target_bir_lowering=False)
    K = Ctx(nc)
    es = contextlib.ExitStack()

    def din(name, shape, dt=F32):
        return nc.dram_tensor(name, list(shape), dt, kind="ExternalInput").ap()

    xb = din("xb", [S, D])
    pq = din("pq", [TQ, 256])
    xq = din("xq", [TQ, D])
    xh = din("xh", [2, D])
    w1 = din("w1", [D, 1024])
    an = din("an", [128, 8])
    wout = din("wout", [D, D])
    wup = din("wup", [D, FF])
    wgate = din("wgate", [D, FF])
    wdown = din("wdown", [FF, D])
    wple = din("wple", [256, D])
    wpg = din("wpg", [D, D])
    fnorm = din("fn", [128, 8])
    pnorm = din("pn", [128, 8])
    cw = din("cw", [128, NF * 3])
    cb = din("cb", [128, NF])
    fin = din("fin", [128, D])
    rn = din("rn", [128, 128])
    hflag = din("hflag", [128, 1])
    c_kst = din("c_kst", [64, S], BF16)
    c_dtab = din("c_dtab", [128, 2 * 128])
    c_negr = din("c_negr", [128, 4])
    c_bdiag = din("c_bdiag", [128, 2 * 2 * 256], BF16)
    c_ekb = din("c_ekb", [128, 4])
    c_decT = din("c_decT", [128, 2 * 256])
    c_qdec = din("c_qdec", [128, 512])
    c_kdec = din("c_kdec", [128, 2])
    c_cd = din("c_cd", [128, 1])
    c_idb = din("c_idb", [128, 128], BF16)
    y = nc.dram_tensor("y", [TQ, D], F32, kind="ExternalOutput").ap()
    dbg = {}
    if DEBUG.get("mix"):
        dbg["mix"] = nc.dram_tensor("dbg_mix", [256, S], BF16, kind="ExternalOutput").ap()

    agi = [nc.dram_tensor("agi%d" % k, [256, SEGT], BF16) for k in range(NSEG)]
    ago = [nc.dram_tensor("ago%d" % k, [1024, SEGT], BF16) for k in range(NSEG)]
    mine = nc.dram_tensor("mine", [1024, NSEG * SEGT], BF16)
    wupg_s = nc.dram_tensor("wupg_s", [NF, 2, 128, 8 * 128], BF16)
    wdown_s = nc.dram_tensor("wdown_s", [128, NF * D], BF16)
    wout_s = nc.dram_tensor("wout_s", [128, 8 * D], BF16)
    wpg_s = nc.dram_tensor("wpg_s", [128, 8 * D], BF16)
    wple_s = nc.dram_tensor("wple_s", [128, 2 * D], BF16)

    def sb(name, shape, dt=F32):
        return es.enter_context(nc.sbuf_tensor(name, list(shape), dt))

    def ps(name, shape, dt=F32):
        return es.enter_context(nc.psum_tensor(name, list(shape), dt))

    trb = [(ps("trb%d" % i, [128, 1024], BF16), Buf("trb%d" % i, True)) for i in range(1)]
    fpb = [(ps("fpb%d" % i, [128, 512], F32), Buf("fpb%d" % i, True)) for i in range(5)]
    obk = [(ps("obk%d" % i, [128, 512], F32), Buf("obk%d" % i, True)) for i in range(2)]
    TR = Rot(trb)
    FP = Rot(fpb[0:2])
    AFP = Rot(fpb[2:5])
    FP2 = Rot(fpb)

    idb = sb("idb", [128, 128], BF16)
    epsr = sb("epsr", [128, 1])
    epsg = sb("epsg", [128, 1])
    zero2 = sb("zero2", [128, 2], BF16)
    K.dma("sp", idb[:], c_idb[:, :], writes=[Buf("c")])
    K.op("dve", lambda e: e.memset(epsr[:], RMS_EPS))
    K.op("dve", lambda e: e.memset(epsg[:], GN_EPS))
    K.op("dve", lambda e: e.memset(zero2[:], 0.0))

    gains = sb("gains", [128, 16])
    gB = Buf("gains")
    K.dma("sp", gains[:, 0:8], fnorm[:, :], writes=[gB])
    K.dma("sp", gains[:, 8:16], pnorm[:, :], writes=[gB])

    with contextlib.ExitStack() as p1:
        def s1(name, shape, dt=F32):
            return p1.enter_context(nc.sbuf_tensor(name, list(shape), dt))

        W1b = s1("W1b", [128, 8, 1024], BF16)
        KA = [s1("KaugA", [128, S], BF16), s1("KaugB", [128, S], BF16)]
        VV = s1("VV", [128, 128, 192], BF16)
        KAB = [[Buf("KaugA%d" % i) for i in range(NG)], [Buf("KaugB%d" % i) for i in range(NG)]]
        VAB = [Buf("VV%d" % i) for i in range(NG)]
        kmT = [s1("kmA", [64, 64], BF16), s1("kmB", [64, 64], BF16)]
        kmB_ = [Buf("kmA"), Buf("kmB")]
        Ctab = s1("Ctab", [128, 128])
        dtab = s1("dtab", [128, 256])
        negr = s1("negr", [128, 4])
        bdiag = s1("bdiag", [128, 2, 2, 256], BF16)
        ekb = s1("ekb", [128, 4])
        decT = s1("decT", [128, 2, 256])
        qdec = s1("qdec", [128, 512])
        kdec = s1("kdec", [128, 2])
        cdc = s1("cdc", [128, 1])
        rng_ = s1("rng", [128, 128])
        an_s = s1("an_s", [128, 8])
        cB = Buf("consts")
        for dst, src in ((dtab[:], c_dtab[:, :]), (negr[:], c_negr[:, :]),
                         (bdiag[:].rearrange("p a b c -> p (a b c)"), c_bdiag[:, :]),
                         (ekb[:], c_ekb[:, :]), (decT[:].rearrange("p a b -> p (a b)"), c_decT[:, :]),
                         (qdec[:], c_qdec[:, :]), (kdec[:], c_kdec[:, :]), (cdc[:], c_cd[:, :]),
                         (rng_[:], rn[:, :]), (an_s[:], an[:, :])):
            K.dma("sp", dst, src, writes=[cB])
        for X in range(2):
            K.dma("sp", KA[X][64:128, :], c_kst[:, :], writes=[cB])
        K.op("dve", lambda e: e.memset(Ctab[:, 0:64], 0.0))
        K.op("dve", lambda e: e.memset(Ctab[:, 64:128], -1e30))
        K.op("pool", lambda e: e.memset(VV[:, :, 64:128], 1.0))
        for X in range(2):
            K.op("dve", lambda e, X=X: e.memset(kmT[X][:], 0.0))
        with contextlib.ExitStack() as pes:
            wst = [(pes.enter_context(nc.sbuf_tensor("wst%d" % i, [128, 1024], F32)), Buf("wst%d" % i)) for i in range(2)]
            for c in range(8):
                st, stB = wst[c % 2]
                K.dma("sp", st[:], w1[c * 128:(c + 1) * 128, :], writes=[stB])
                K.op("dve", lambda e, st=st, c=c: e.tensor_scalar(out=W1b[:, c, :], in0=st[:], scalar1=an_s[:, c:c + 1],
                                                                  scalar2=None, op0=ALU.mult), reads=[stB, cB])
            K.barrier()

        mbin = [[[(s1("mbin%d%d%d" % (X, par, i), [128, 128], BF16), Buf("mbin")) for i in range(2)]
                 for par in range(2)] for X in range(2)]
        for X in range(2):
            for par in range(2):
                for i in range(2):
                    t_, _ = mbin[X][par][i]
                    K.op("dve", lambda e, t_=t_: e.memset(t_[:, 0:64], 0.0))
                    K.op("dve", lambda e, t_=t_, X=X, par=par: e.tensor_copy(out=t_[:, 127:128],
                                                                             in_=negr[:, 2 * X + par:2 * X + par + 1]))
        xt = [(s1("xt%d" % i, [128, 1024]), Buf("xt%d" % i)) for i in range(2)]
        ssb = [(s1("ss%d" % i, [128, 1]), Buf("ss")) for i in range(4)]
        rsb = [(s1("rs%d" % i, [128, 1]), Buf("rs")) for i in range(4)]
        xs = [(s1("xs%d" % i, [128, 1024], BF16), Buf("xs%d" % i)) for i in range(2)]
        hT = [(s1("hT%d" % i, [128, 8, 512], BF16), Buf("hT%d" % i)) for i in range(2)]
        QA = [[(s1("Qaug%d%d" % (X, i), [128, 512], BF16), Buf("Qaug")) for i in range(2)] for X in range(2)]
        kms = [(s1("kms%d" % i, [128, 2]), Buf("kms")) for i in range(2)]
        Gm = [(s1("Gm%d" % i, [128, 64]), Buf("Gm")) for i in range(2)]
        m8 = [(s1("m8%d" % i, [128, 8]), Buf("m8")) for i in range(2)]
        thr = [(s1("thr%d" % i, [128, 1]), Buf("thr")) for i in range(2)]
        mb32 = [(s1("mb32%d" % i, [128, 64]), Buf("mb32")) for i in range(2)]
        PT = Rot([(s1("PT%d" % i, [128, 512], BF16), Buf("PT%d" % i)) for i in range(3)])
        rc0 = (s1("rc0", [128, 512]), Buf("rc"))
        rc = [rc0, rc0]
        mixst = [(s1("mixst%d" % i, [128, 512], BF16), Buf("mixst%d" % i)) for i in range(2)]
        retst = [(s1("retst%d" % i, [128, 512], BF16), Buf("retst%d" % i)) for i in range(2)]
        rqT = [(s1("rqT%d" % i, [128, 512], BF16), Buf("rqT")) for i in range(2)]
        rqTd = [(s1("rqTd%d" % i, [128, 512], BF16), Buf("rqTd")) for i in range(2)]
        rkT = [(s1("rkT%d" % i, [128, 512], BF16), Buf("rkT")) for i in range(2)]
        rkd = [(s1("rkd%d" % i, [128, 4, 128], BF16), Buf("rkd")) for i in range(2)]
        rvt = [(s1("rvt%d" % i, [128, 4, 128], BF16), Buf("rvt")) for i in range(2)]
        sg = [(s1("sg%d" % i, [128, 4, 128]), Buf("sg")) for i in range(2)]
        gs = sg
        AT = Rot([(s1("AT%d" % i, [128, 2, 256], BF16), Buf("AT")) for i in range(2)])
        st6 = Rot([(s1("st6%d" % i, [128, 6]), Buf("st6")) for i in range(2)])
        mv = Rot([(s1("mv%d" % i, [128, 2]), Buf("mv")) for i in range(2)])
        rstd2 = Rot([(s1("rstd2%d" % i, [128, 1]), Buf("rstd2")) for i in range(2)])
        yn = Rot([(s1("yn%d" % i, [128, 128]), Buf("yn")) for i in range(2)])
        ynb = Rot([(s1("ynb%d" % i, [128, 128], BF16), Buf("ynb")) for i in range(2)])
        state = s1("state", [128, 128])
        stateB = Buf("state")
        stbf = Rot([(s1("stbf%d" % i, [128, 128], BF16), Buf("stbf")) for i in range(2)])
        K.op("dve", lambda e: e.memset(state[:], 0.0), writes=[stateB])
        cur_st = stbf.next()
        K.op("dve", lambda e: e.memset(cur_st[0][:], 0.0), writes=[cur_st[1]])
        K.barrier()

        ALT = [0]

        def evac_eng():
            ALT[0] += 1
            return "act" if ALT[0] % 2 else "dve"

        def copy_op(eng, out, in_, reads, writes, scale=None):
            if eng == "act":
                if scale is None:
                    return K.op("act", lambda e: e.copy(out=out, in_=in_), reads=reads, writes=writes)
                return K.op("act", lambda e: e.mul(out=out, in_=in_, mul=scale), reads=reads, writes=writes)
            if scale is None:
                return K.op(eng, lambda e: e.tensor_copy(out=out, in_=in_), reads=reads, writes=writes)
            return K.op(eng, lambda e: e.tensor_scalar(out=out, in0=in_, scalar1=scale, scalar2=None, op0=ALU.mult),
                        reads=reads, writes=writes)

        def prep(g):
            h, hB = hT[g % 2]
            for t in range(4):
                T = 4 * g + t
                x_, xB = xt[T % 2]
                K.dma("sp", x_[:], xb[T * 128:(T + 1) * 128, :], writes=[xB])
                ss_, ssB = ssb[T % 4]
                rs_, rsB = rsb[T % 4]
                xs_, xsB = xs[T % 2]
                K.op("act", lambda e: e.activation(out=xs_[:], in_=x_[:], func=AF.Square, accum_out=ss_[:]),
                     reads=[xB], writes=[ssB, xsB])
                K.op("act", lambda e: e.activation(out=rs_[:], in_=ss_[:], func=AF.Ln, bias=epsr[:, 0:1], scale=1.0 / D),
                     reads=[ssB], writes=[rsB])
                K.op("act", lambda e: e.activation(out=rs_[:], in_=rs_[:], func=AF.Exp, scale=-0.5), reads=[rsB], writes=[rsB])
                yield
                xs_, xsB = xs[T % 2]
                K.op("dve", lambda e: e.tensor_scalar(out=xs_[:], in0=x_[:], scalar1=rs_[:, 0:1], scalar2=None, op0=ALU.mult),
                     reads=[xB, rsB], writes=[xsB])
                yield
                tr, trB = TR.next()
                for c in range(8):
                    K.op("pe", lambda e, c=c: e.transpose(out=tr[:, c * 128:(c + 1) * 128], in_=xs_[:, c * 128:(c + 1) * 128],
                                                          identity=idb[:]), reads=[xsB], writes=[trB])
                copy_op("dve", h[:, :, t * 128:(t + 1) * 128], tr[:].rearrange("p (c j) -> p c j", j=128),
                        reads=[trB], writes=[hB])
                yield

        def proj(g, js=(0, 1, 2, 3), do_tm=True):
            h, hB = hT[g % 2]
            for j in js:
                bk, bkB = fpb[0]
                for c in range(8):
                    K.op("pe", lambda e, c=c: e.matmul(bk[:], lhsT=W1b[:, c, j * 128:(j + 1) * 128], rhs=h[:, c, :],
                                                       start=(c == 0), stop=(c == 7)), reads=[hB], writes=[bkB])
                yield
                if j == 0:
                    for X in range(2):
                        q_, qB = QA[X][g % 2]
                        copy_op("act" if X == 0 else "dve", q_[0:64, :], bk[64 * X:64 * X + 64, :], [bkB], [qB], scale=0.125)
                elif j == 1:
                    for X in range(2):
                        copy_op("act" if X == 0 else "dve", KA[X][0:64, g * 512:(g + 1) * 512], bk[64 * X:64 * X + 64, :],
                                [bkB], [KAB[X][g]])
                    km_, kmsB = kms[g % 2]
                    K.op("dve", lambda e: e.tensor_reduce(out=km_[:], in_=bk[:].rearrange("p (a b) -> p a b", b=256),
                                                          axis=AX.X, op=ALU.add), reads=[bkB], writes=[kmsB])
                    for X in range(2):
                        K.op("dve", lambda e, X=X: e.tensor_scalar(out=kmT[X][0:64, 2 * g:2 * g + 2], in0=km_[64 * X:64 * X + 64, :],
                                                                   scalar1=1.0 / 256, scalar2=None, op0=ALU.mult),
                             reads=[kmsB], writes=[kmB_[X]])
                elif j == 2:
                    copy_op("act", rqT[g % 2][0][:], bk[:], [bkB], [rqT[g % 2][1]])
                    K.op("dve", lambda e: e.tensor_tensor(out=rqTd[g % 2][0][:], in0=bk[:], in1=qdec[:], op=ALU.mult),
                         reads=[bkB], writes=[rqTd[g % 2][1]])
                else:
                    copy_op("act", rkT[g % 2][0][:], bk[:], [bkB], [rkT[g % 2][1]], scale=128 ** -0.5)
                yield
            for t in (range(4) if do_tm else ()):
                bk, bkB = fpb[0]
                kc = 4 * g + t
                for c in range(8):
                    K.op("pe", lambda e, c=c: e.matmul(bk[:], lhsT=h[:, c, t * 128:(t + 1) * 128], rhs=W1b[:, c, 512:1024],
                                                       start=(c == 0), stop=(c == 7)), reads=[hB], writes=[bkB])
                yield
                copy_op("act", VV[:, kc, 0:64], bk[:, 0:64], [bkB], [VAB[g]])
                copy_op("dve", VV[:, kc, 128:192], bk[:, 64:128], [bkB], [VAB[g]])
                K.op("dve", lambda e: e.tensor_scalar(out=rkd[g % 2][0][:, t, :], in0=bk[:, 128:256],
                                                      scalar1=kdec[:, t % 2:t % 2 + 1], scalar2=None, op0=ALU.mult),
                     reads=[bkB], writes=[rkd[g % 2][1]])
                copy_op("act", rvt[g % 2][0][:, t, :], bk[:, 256:384], [bkB], [rvt[g % 2][1]])
                K.op("act", lambda e: e.activation(out=sg[g % 2][0][:, t, :], in_=bk[:, 384:512], func=AF.Exp, scale=-1.0),
                     reads=[bkB], writes=[sg[g % 2][1]])
                K.op("dve", lambda e: e.tensor_scalar_add(out=sg[g % 2][0][:, t, :], in0=sg[g % 2][0][:, t, :], scalar1=1.0),
                     reads=[sg[g % 2][1]], writes=[sg[g % 2][1]])
                K.op("dve", lambda e: e.reciprocal(out=sg[g % 2][0][:, t, :], in_=sg[g % 2][0][:, t, :]),
                     reads=[sg[g % 2][1]], writes=[sg[g % 2][1]])
                K.op("dve", lambda e: e.tensor_tensor(out=sg[g % 2][0][:, t, :], in0=sg[g % 2][0][:, t, :], in1=bk[:, 384:512], op=ALU.mult),
                     reads=[bkB, sg[g % 2][1]], writes=[sg[g % 2][1]])
                K.op("dve", lambda e: e.tensor_tensor(out=sg[g % 2][0][:, t, :], in0=sg[g % 2][0][:, t, :], in1=rng_[:],
                                                      op=ALU.mult), reads=[sg[g % 2][1]], writes=[sg[g % 2][1]])
                yield

        MI = [0]

        def masks(g):
            bk, bkB = fpb[0]
            for t in range(4):
                for X in range(2):
                    q_, qB = QA[X][g % 2]
                    o = (4 * X + t) * 64
                    K.op("pe", lambda e: e.matmul(bk[:, o:o + 64], lhsT=q_[0:64, t * 128:(t + 1) * 128], rhs=kmT[X][0:64, 0:64],
                                                  start=True, stop=True), reads=[qB, kmB_[X]], writes=[bkB])
            yield
            mbis = []
            for t in range(4):
                a = 2 * g + t // 2
                par = t % 2
                for X in range(2):
                    o = (4 * X + t) * 64
                    i = MI[0] % 2
                    MI[0] += 1
                    K.op("dve", lambda e: e.tensor_tensor(out=Gm[i][0][:], in0=bk[:, o:o + 64], in1=Ctab[:, 64 - a:128 - a], op=ALU.add),
                         reads=[bkB], writes=[Gm[i][1]])
                    K.op("dve", lambda e: e.max(out=m8[i][0][:], in_=Gm[i][0][:]), reads=[Gm[i][1]], writes=[m8[i][1]])
                    K.op("dve", lambda e: e.tensor_scalar_max(out=thr[i][0][:], in0=m8[i][0][:, 2:3], scalar1=-1e29),
                         reads=[m8[i][1]], writes=[thr[i][1]])
                    K.op("dve", lambda e: e.tensor_scalar(out=mb32[i][0][:], in0=Gm[i][0][:], scalar1=thr[i][0][:, 0:1],
                                                          scalar2=BIG, op0=ALU.is_ge, op1=ALU.mult),
                         reads=[Gm[i][1], thr[i][1]], writes=[mb32[i][1]])
                    mbi, mbiB = mbin[X][par][(t // 2) % 2]
                    K.op("dve", lambda e: e.tensor_tensor(out=mbi[:, 64:127], in0=mb32[i][0][:, 0:63],
                                                          in1=dtab[:, 128 * X + 64 - a:128 * X + 127 - a], op=ALU.add),
                         reads=[mb32[i][1]], writes=[mbiB])
                    mbis.append((t, X, mbi, mbiB))
                    yield
            tr, trB = TR.next()
            for (t, X, mbi, mbiB) in mbis:
                o = (4 * X + t) * 128
                K.op("pe", lambda e: e.transpose(out=tr[:, o:o + 128], in_=mbi[:], identity=idb[:]), reads=[mbiB], writes=[trB])
            for X in range(2):
                q_, qB = QA[X][g % 2]
                copy_op("act" if X == 0 else "dve", q_[64:128, :], tr[64:128, X * 512:(X + 1) * 512], [trB], [qB])
            yield

        def attn(g, side):
            items = []
            firsts = [[True], [True]]

            def mk_pv(X, kc, lo, hi, ptref, last=False):
                ob, obB = obk[X]
                vlo = 64 * X

                def f():
                    pt, ptB = ptref[0]
                    K.op("pe", lambda e: e.matmul(ob[:, lo:hi], lhsT=VV[:, kc, vlo:vlo + 128], rhs=pt[:, lo:hi],
                                                  start=firsts[X][0], stop=last), reads=[ptB, VAB[kc // 4]], writes=[obB])
                    firsts[X][0] = False
                return f

            def past_item(X, kc, c):
                q_, qB = QA[X][g % 2]
                sref, ptref = [None], [None]

                def qk():
                    sref[0] = AFP.next()
                    sbk, sbB = sref[0]
                    K.op("pe", lambda e: e.matmul(sbk[:], lhsT=KA[X][:, kc * 128:(kc + 1) * 128], rhs=q_[:, :], start=True, stop=True),
                         reads=[qB, KAB[X][kc // 4]], writes=[sbB])

                def ex():
                    sbk, sbB = sref[0]
                    ptref[0] = PT.next()
                    pt, ptB = ptref[0]
                    K.op("act", lambda e: e.activation(out=pt[:], in_=sbk[:], func=AF.Exp, bias=ekb[:, 2 * X + c:2 * X + c + 1], scale=1.0),
                         reads=[sbB], writes=[ptB])
                return (qk, ex, mk_pv(X, kc, 0, 512, ptref))

            def diag0_item(X, c):
                q_, qB = QA[X][g % 2]
                kc = 4 * g + c
                q0 = 0 if c == 0 else 128
                sref, ptref = [None], [None]

                def qk():
                    sref[0] = AFP.next()
                    sbk, sbB = sref[0]
                    K.op("pe", lambda e: e.matmul(sbk[:, q0:256], lhsT=KA[X][0:64, kc * 128:(kc + 1) * 128], rhs=q_[0:64, q0:256],
                                                  start=True, stop=False), reads=[qB, KAB[X][kc // 4]], writes=[sbB])
                    K.op("pe", lambda e: e.matmul(sbk[:, q0:256], lhsT=idb[:], rhs=bdiag[:, X, c, q0:256], start=False, stop=True),
                         writes=[sbB])
                    K.op("pe", lambda e: e.matmul(sbk[:, 256:512], lhsT=KA[X][:, kc * 128:(kc + 1) * 128], rhs=q_[:, 256:512],
                                                  start=True, stop=True), reads=[qB, KAB[X][kc // 4]], writes=[sbB])

                def ex():
                    sbk, sbB = sref[0]
                    ptref[0] = PT.next()
                    pt, ptB = ptref[0]
                    K.op("act", lambda e: e.activation(out=pt[:, q0:256], in_=sbk[:, q0:256], func=AF.Exp), reads=[sbB], writes=[ptB])
                    K.op("act", lambda e: e.activation(out=pt[:, 256:512], in_=sbk[:, 256:512], func=AF.Exp,
                                                       bias=ekb[:, 2 * X + c:2 * X + c + 1], scale=1.0), reads=[sbB], writes=[ptB])
                return (qk, ex, mk_pv(X, kc, q0, 512, ptref))

            def diag1_item(X, c):
                q_, qB = QA[X][g % 2]
                kc = 4 * g + 2 + c
                q0 = 256 if c == 0 else 384
                sref, ptref = [None], [None]

                def qk():
                    sref[0] = AFP.next()
                    sbk, sbB = sref[0]
                    K.op("pe", lambda e: e.matmul(sbk[:, q0:512], lhsT=KA[X][0:64, kc * 128:(kc + 1) * 128], rhs=q_[0:64, q0:512],
                                                  start=True, stop=False), reads=[qB, KAB[X][kc // 4]], writes=[sbB])
                    K.op("pe", lambda e: e.matmul(sbk[:, q0:512], lhsT=idb[:], rhs=bdiag[:, X, c, q0 - 256:256], start=False, stop=True),
                         writes=[sbB])

                def ex():
                    sbk, sbB = sref[0]
                    ptref[0] = PT.next()
                    pt, ptB = ptref[0]
                    K.op("act", lambda e: e.activation(out=pt[:, q0:512], in_=sbk[:, q0:512], func=AF.Exp), reads=[sbB], writes=[ptB])
                return (qk, ex, mk_pv(X, kc, q0, 512, ptref, last=(c == 1)))

            if g == 0:
                for c in range(2):
                    for X in range(2):
                        items.append(diag0_item(X, c))
            for n in range(2 * g):
                for c in range(2):
                    for X in range(2):
                        if X == 0 and n < 2 * g - WIN0:
                            continue
                        items.append(past_item(X, 2 * n + c, c))
            if g > 0:
                for c in range(2):
                    for X in range(2):
                        items.append(diag0_item(X, c))
            for c in range(2):
                for X in range(2):
                    items.append(diag1_item(X, c))
            LA = 2
            stride = max(1, len(items) // 80)
            burst = max(1, -(-80 // len(items)))
            for i in range(len(items) + LA):
                if i < len(items):
                    items[i][0]()
                j = i - LA
                if j >= 0:
                    items[j][1]()
                    items[j][2]()
                if i % stride == 0:
                    for _ in range(burst):
                        next(side, None)
            for _ in side:
                pass
            for X in range(2):
                ob, obB = obk[X]
                r_, rB = rc[X]
                m_, mB = mixst[g % 2]
                nlo, dlo = (0, 64) if X == 0 else (64, 0)
                K.op("dve", lambda e: e.reciprocal(out=r_[nlo:nlo + 64, :], in_=ob[dlo:dlo + 64, :]), reads=[obB], writes=[rB])
                K.op("dve", lambda e: e.tensor_tensor(out=m_[nlo:nlo + 64, :], in0=ob[nlo:nlo + 64, :], in1=r_[nlo:nlo + 64, :], op=ALU.mult),
                     reads=[obB, rB], writes=[mB])

        CUR = [cur_st]

        def ret(g):
            i2 = g % 2
            rq_, rqB = rqT[i2]
            rqd_, rqdB = rqTd[i2]
            rk_, rkB = rkT[i2]
            rkd_, rkdB = rkd[i2]
            rv_, rvB = rvt[i2]
            gs_, gsB = gs[i2]
            rst, rstB = retst[i2]
            for ch in range(2):
                c0 = 256 * ch
                at, atB = AT.next()
                sbk, sbB = fpb[1]
                ub, ubB = fpb[1]
                K.op("pe", lambda e: e.matmul(sbk[:, 0:256], lhsT=rk_[:, c0:c0 + 128], rhs=rq_[:, c0:c0 + 256],
                                              start=True, stop=True), reads=[rkB, rqB], writes=[sbB])
                K.op("pe", lambda e: e.matmul(sbk[:, 256:384], lhsT=rk_[:, c0 + 128:c0 + 256], rhs=rq_[:, c0 + 128:c0 + 256],
                                              start=True, stop=True), reads=[rkB, rqB], writes=[sbB])
                for kh in range(2):
                    K.op("pe", lambda e: e.matmul(ub[:, 384:512], lhsT=rkd_[:, 2 * ch + kh, :], rhs=rv_[:, 2 * ch + kh, :],
                                                  start=(kh == 0), stop=(kh == 1)), reads=[rkdB, rvB], writes=[ubB])
                yield
                K.op("dve", lambda e: e.tensor_tensor(out=at[:, 0, 0:256], in0=sbk[:, 0:256], in1=decT[:, 0, 0:256], op=ALU.mult),
                     reads=[sbB], writes=[atB])
                K.op("dve", lambda e: e.tensor_tensor(out=at[:, 1, 128:256], in0=sbk[:, 256:384], in1=decT[:, 1, 128:256], op=ALU.mult),
                     reads=[sbB], writes=[atB])
                st_, stB_ = CUR[0]
                K.op("dve", lambda e: e.scalar_tensor_tensor(out=state[:], in0=state[:], scalar=cdc[:, 0:1], in1=ub[:, 384:512],
                                                             op0=ALU.mult, op1=ALU.add), reads=[ubB, stateB], writes=[stateB])
                nst = stbf.next()
                K.op("dve", lambda e: e.tensor_copy(out=nst[0][:], in_=state[:]), reads=[stateB], writes=[nst[1]])
                CUR[0] = nst
                yield
                rb, rbB = fpb[1]
                for qt in range(2):
                    o = 128 * qt
                    K.op("pe", lambda e: e.matmul(rb[:, o:o + 128], lhsT=at[:, 0, qt * 128:(qt + 1) * 128], rhs=rv_[:, 2 * ch, :],
                                                  start=True, stop=False), reads=[atB, rvB], writes=[rbB])
                    if qt == 1:
                        K.op("pe", lambda e: e.matmul(rb[:, o:o + 128], lhsT=at[:, 1, 128:256], rhs=rv_[:, 2 * ch + 1, :],
                                                      start=False, stop=False), reads=[atB, rvB], writes=[rbB])
                    K.op("pe", lambda e: e.matmul(rb[:, o:o + 128], lhsT=rqd_[:, c0 + qt * 128:c0 + qt * 128 + 128], rhs=st_[:],
                                                  start=False, stop=True), reads=[rqdB, stB_], writes=[rbB])
                yield
                ybs = []
                for qt in range(2):
                    o = 128 * qt
                    s6, s6B = st6.next()
                    mv_, mvB = mv.next()
                    r2, r2B = rstd2.next()
                    y_, yB = yn.next()
                    yb_, ybB = ynb.next()
                    K.op("dve", lambda e: e.bn_stats(out=s6[:], in_=rb[:, o:o + 128]), reads=[rbB], writes=[s6B])
                    K.op("dve", lambda e: e.bn_aggr(out=mv_[:], in_=s6[:]), reads=[s6B], writes=[mvB])
                    K.op("act", lambda e: e.activation(out=r2[:], in_=mv_[:, 1:2], func=AF.Ln, bias=epsg[:, 0:1], scale=1.0),
                         reads=[mvB], writes=[r2B])
                    K.op("act", lambda e: e.activation(out=r2[:], in_=r2[:], func=AF.Exp, scale=-0.5), reads=[r2B], writes=[r2B])
                    yield
                    K.op("dve", lambda e: e.tensor_scalar(out=y_[:], in0=rb[:, o:o + 128], scalar1=mv_[:, 0:1], scalar2=r2[:, 0:1],
                                                          op0=ALU.subtract, op1=ALU.mult), reads=[rbB, mvB, r2B], writes=[yB])
                    K.op("dve", lambda e: e.tensor_tensor(out=yb_[:], in0=y_[:], in1=gs_[:, 2 * ch + qt, :], op=ALU.mult),
                         reads=[yB, gsB], writes=[ybB])
                    ybs.append((yb_, ybB))
                    yield
                tr, trB = TR.next()
                for qt in range(2):
                    K.op("pe", lambda e: e.transpose(out=tr[:, qt * 128:(qt + 1) * 128], in_=ybs[qt][0][:], identity=idb[:]),
                         reads=[ybs[qt][1]], writes=[trB])
                copy_op("dve", rst[:, c0:c0 + 256], tr[:, 0:256], [trB], [rstB])
                yield

        agiB = [Buf("agi%d" % k) for k in range(NSEG)]
        agsem = [K.newsem("ag") for _ in range(NSEG)]
        mineB = Buf("mine")

        def outp(g):
            k = g // 4
            c0 = (g % 4) * 512
            m_, mB = mixst[g % 2]
            r_, rB = retst[g % 2]
            K.dma("sp", agi[k].ap()[0:128, c0:c0 + 512], m_[:], reads=[mB], writes=[agiB[k]])
            K.dma("sp", agi[k].ap()[128:256, c0:c0 + 512], r_[:], reads=[rB], writes=[agiB[k]])
            if "mix" in dbg:
                K.dma("sp", dbg["mix"][0:128, g * 512:(g + 1) * 512], m_[:], reads=[mB])
                K.dma("sp", dbg["mix"][128:256, g * 512:(g + 1) * 512], r_[:], reads=[rB])
            if g % 4 == 3:
                K.deps("pool", [agiB[k]], [])
                nc.gpsimd.collective_compute(
                    "AllGather", ALU.bypass, replica_groups=[[0, 1, 2, 3], [4, 5, 6, 7]],
                    ins=[agi[k].ap().opt()], outs=[ago[k].ap().opt()],
                ).then_inc(agsem[k].h)
                agsem[k].v = 1

        QV = [None]

        def agcopy(k):
            nc.gpsimd.wait_ge(agsem[k].h, 1)
            if QV[0] is None:
                QV[0] = nc.gpsimd.snap(nc.gpsimd.partition_id() % 4)
            qv = QV[0]
            slot = (qv * 6 + k) % 8
            K.dma("pool", mine.ap()[:, bass.ds(slot * SEGT, SEGT)], ago[k].ap()[:, :], writes=[mineB])

        import itertools
        PW = 512
        wst_f = [(s1("wcf%d" % i, [128, PW]), Buf("wcf%d" % i)) for i in range(2)]
        wst_b = [(s1("wcb%d" % i, [128, PW], BF16), Buf("wcb%d" % i)) for i in range(2)]
        WCN = [0]

        def wpiece(src, dst, dst3, gain, n=PW):
            i = WCN[0] % 2
            WCN[0] += 1
            (st, stB), (sbf, sbB) = wst_f[i], wst_b[i]
            K.dma("sp", st[:, 0:n], src, writes=[stB])
            if gain is None:
                K.op("act", lambda e: e.copy(out=sbf[:, 0:n], in_=st[:, 0:n]), reads=[stB], writes=[sbB])
            else:
                K.op("act", lambda e: e.activation(out=sbf[:, 0:n], in_=st[:, 0:n], func=AF.Identity, scale=gain),
                     reads=[stB], writes=[sbB])
            K.dma("pool", dst, sbf[:, 0:n].rearrange("p (f j) -> p f j", j=128) if dst3 else sbf[:, 0:n], reads=[sbB])

        def wconv():
            for c in range(8):
                rows = slice(c * 128, (c + 1) * 128)
                for wi, wsrc in enumerate((wup, wgate)):
                    for fo in range(0, NF, 4):
                        nf = min(4, NF - fo)
                        dst = wupg_s.ap()[fo:fo + nf, wi, :, c * 128:(c + 1) * 128].rearrange("f p j -> p f j")
                        wpiece(wsrc[rows, fo * 128:(fo + nf) * 128], dst, True, gains[:, c:c + 1], n=nf * 128)
                        yield
                for k4 in range(D // PW):
                    wpiece(wout[rows, k4 * PW:(k4 + 1) * PW], wout_s.ap()[:, c * D + k4 * PW:c * D + (k4 + 1) * PW], False, None)
                    yield
                    wpiece(wpg[rows, k4 * PW:(k4 + 1) * PW], wpg_s.ap()[:, c * D + k4 * PW:c * D + (k4 + 1) * PW], False,
                           gains[:, 8 + c:9 + c])
                    yield
            for f in range(NF):
                for k4 in range(D // PW):
                    wpiece(wdown[f * 128:(f + 1) * 128, k4 * PW:(k4 + 1) * PW],
                           wdown_s.ap()[:, f * D + k4 * PW:f * D + (k4 + 1) * PW], False, None)
                    yield
            for c in range(2):
                for k4 in range(D // PW):
                    wpiece(wple[c * 128:(c + 1) * 128, k4 * PW:(k4 + 1) * PW],
                           wple_s.ap()[:, c * D + k4 * PW:c * D + (k4 + 1) * PW], False, None)
                    yield
        WG = wconv()
        NPIECE = 8 * (2 * 6 + 4) + NF * 2 + 4
        PER_G = -(-NPIECE // NG)

        def run(gen):
            for _ in gen:
                pass

        def roundrobin(gens):
            live = list(gens)
            while live:
                for g_ in list(live):
                    if next(g_, "end") == "end":
                        live.remove(g_)
                    else:
                        yield
        run(prep(0))
        run(proj(0))
        run(masks(0))
        run(prep(1))
        for g in range(NG):
            side = [ret(g)]
            if g + 1 < NG:
                side += [itertools.chain(proj(g + 1, (0, 1), False), masks(g + 1), proj(g + 1, (2, 3), True))]
            if g + 2 < NG:
                side += [prep(g + 2)]
            side += [itertools.islice(WG, PER_G)]
            attn(g, roundrobin(side))
            outp(g)
            if g % 4 == 1 and g >= 5:
                agcopy((g - 5) // 4)
        run(WG)
        agcopy(NSEG - 1)
        K.barrier()

    with contextlib.ExitStack() as p2:
        def s2(name, shape, dt=F32):
            return p2.enter_context(nc.sbuf_tensor(name, list(shape), dt))

        wdn = s2("wdn", [128, NF, D], BF16)
        wo = s2("wo", [128, 8, D], BF16)
        wg = s2("wg", [128, 8, D], BF16)
        wp = s2("wp", [128, 2, D], BF16)
        cws = s2("cws", [128, NF, 3])
        cbs = s2("cbs", [128, NF])
        fins = s2("fins", [128, D])
        hfl = s2("hfl", [128, 1])
        cB = Buf("c2")
        K.dma("sp", wdn[:].rearrange("p f n -> p (f n)"), wdown_s.ap()[:, :], writes=[cB])
        K.dma("sp", wo[:].rearrange("p f n -> p (f n)"), wout_s.ap()[:, :], writes=[cB])
        K.dma("sp", wg[:].rearrange("p f n -> p (f n)"), wpg_s.ap()[:, :], writes=[cB])
        K.dma("sp", wp[:].rearrange("p f n -> p (f n)"), wple_s.ap()[:, :], writes=[cB])
        K.dma("sp", cws[:].rearrange("p f n -> p (f n)"), cw[:, :], writes=[cB])
        K.dma("sp", cbs[:], cb[:, :], writes=[cB])
        K.dma("sp", fins[:], fin[:, :], writes=[cB])
        K.dma("sp", hfl[:], hflag[:, :], writes=[cB])

        mixT = [(s2("mixT%d" % i, [128, 8, 512], BF16), Buf("mixT%d" % i)) for i in range(2)]
        xr = s2("xr", [128, 4, D])
        xrB = [Buf("xr%d" % t) for t in range(4)]
        pin = [(s2("pin%d" % i, [128, 256]), Buf("pin%d" % i)) for i in range(2)]
        pbf = [(s2("pbf%d" % i, [128, 256], BF16), Buf("pbf")) for i in range(2)]
        pT = s2("pT", [128, 2, 512], BF16)
        pTB = Buf("pT")
        junk2 = s2("junk2", [128, D], BF16)
        ss2 = Rot([(s2("ss2%d" % i, [128, 1]), Buf("ss2")) for i in range(4)])
        xs2 = Rot([(s2("xs2%d" % i, [128, D], BF16), Buf("xs2")) for i in range(2)])
        hT2 = s2("hT2", [128, 8, 512], BF16)
        hT2B = Buf("hT2")
        actT = s2("actT", [128, NF, 512], BF16)
        actB = [Buf("act%d" % f) for f in range(NF)]
        uh = s2("uh", [128, NF, 2])
        uhB = [Buf("uh%d" % f) for f in range(NF)]
        usb = Rot([(s2("usb%d" % i, [128, 514]), Buf("usb")) for i in range(2)])
        yc = Rot([(s2("yc%d" % i, [128, 512]), Buf("yc")) for i in range(2)])
        gl = Rot([(s2("gl%d" % i, [128, 512]), Buf("gl")) for i in range(2)])
        WS = Rot([(s2("ws%d" % i, [128, 2, 8 * 128], BF16), Buf("ws%d" % i)) for i in range(3)])
        sgm = Rot([(s2("sgm%d" % i, [128, 512]), Buf("sgm")) for i in range(2)])
        yo = Rot([(s2("yo%d" % i, [128, D]), Buf("yo%d" % i)) for i in range(2)])
        xhs = s2("xhs", [2, D])
        xhsB = Buf("xhs")
        mixh = s2("mixh", [128, 8, 2], BF16)
        mixhB = Buf("mixh")
        hhT = s2("hhT", [128, 8, 2], BF16)
        hhTB = Buf("hhT")
        xsh = s2("xsh", [2, D], BF16)
        xshB = Buf("xsh")
        ssh = s2("ssh", [2, 1])
        sshB = Buf("ssh")
        for f in range(NF):
            pass
        K.barrier()

        def rms_to_hT(t, xin_ap, xin_B, dst, dstB, npart=128):
            ss_, ssB = ss2.next()
            xs_, xsB = xs2.next()
            K.op("act", lambda e: e.activation(out=junk2[0:npart, :], in_=xin_ap, func=AF.Square, accum_out=ss_[0:npart, :]),
                 reads=[xin_B], writes=[ssB])
            K.op("act", lambda e: e.activation(out=ss_[0:npart, :], in_=ss_[0:npart, :], func=AF.Ln, bias=epsr[0:npart, 0:1], scale=1.0 / D),
                 reads=[ssB], writes=[ssB])
            K.op("act", lambda e: e.activation(out=ss_[0:npart, :], in_=ss_[0:npart, :], func=AF.Exp, scale=-0.5), reads=[ssB], writes=[ssB])
            K.op("dve", lambda e: e.tensor_scalar(out=xs_[0:npart, :], in0=xin_ap, scalar1=ss_[0:npart, 0:1], scalar2=None, op0=ALU.mult),
                 reads=[xin_B, ssB], writes=[xsB])
            tr, trB = TR.next()
            for c in range(8):
                K.op("pe", lambda e, c=c: e.transpose(out=tr[:, c * 128:c * 128 + npart], in_=xs_[0:npart, c * 128:(c + 1) * 128],
                                                      identity=idb[0:npart, 0:npart]), reads=[xsB], writes=[trB])
            if npart == 128:
                copy_op(evac_eng(), dst[:, :, t * 128:(t + 1) * 128], tr[:].rearrange("p (c j) -> p c j", j=128), [trB], [dstB])
            else:
                copy_op("dve", dst[:, :, 0:npart], tr[:].rearrange("p (c j) -> p c j", j=128)[:, :, 0:npart], [trB], [dstB])
            return ss_, ssB

        def load_mix(j, buf):
            m_, mB = buf
            seg = j // 4
            c0 = (j % 4) * 512
            K.dma("sp", m_[:], mine.ap()[:, seg * SEGT + c0:seg * SEGT + c0 + 512].rearrange("(c p) t -> p c t", p=128), reads=[mineB], writes=[mB])

        K.dma("sp", xhs[:], xh[:, :], writes=[xhsB])
        K.dma("sp", mixh[:], mine.ap()[:, 8 * SEGT - 2:8 * SEGT].rearrange("(c p) t -> p c t", p=128), reads=[mineB], writes=[mixhB])
        for hf in range(2):
            bk, bkB = FP2.next()
            for c in range(8):
                K.op("pe", lambda e, c=c: e.matmul(bk[0:2, :], lhsT=mixh[:, c, :], rhs=wo[:, c, hf * 512:(hf + 1) * 512],
                                                   start=(c == 0), stop=(c == 7)), reads=[mixhB], writes=[bkB])
            K.op("dve", lambda e: e.tensor_tensor(out=xhs[:, hf * 512:(hf + 1) * 512], in0=bk[0:2, :], in1=xhs[:, hf * 512:(hf + 1) * 512],
                                                  op=ALU.add), reads=[bkB, xhsB], writes=[xhsB])
        rms_to_hT(0, xhs[:], xhsB, hhT, hhTB, npart=2)

        load_mix(0, mixT[0])
        for j in range(NT2):
            m_, mB = mixT[j % 2]
            if j + 1 < NT2:
                load_mix(j + 1, mixT[(j + 1) % 2])
            for t in range(4):
                r0 = j * 512 + t * 128
                K.dma("sp", xr[:, t, :], xq[r0:r0 + 128, :], writes=[xrB[t]])
                p_, pB = pin[t % 2]
                K.dma("sp", p_[:], pq[r0:r0 + 128, :], writes=[pB])
                pb_, pbB = pbf[t % 2]
                K.op("pool", lambda e: e.tensor_copy(out=pb_[:], in_=p_[:]), reads=[pB], writes=[pbB])
                tr, trB = TR.next()
                for c in range(2):
                    K.op("pe", lambda e, c=c: e.transpose(out=tr[:, c * 128:(c + 1) * 128], in_=pb_[:, c * 128:(c + 1) * 128],
                                                          identity=idb[:]), reads=[pbB], writes=[trB])
                copy_op(evac_eng(), pT[:, :, t * 128:(t + 1) * 128], tr[:, 0:256].rearrange("p (c j) -> p c j", j=128), [trB], [pTB])
                for hf in range(2):
                    bk, bkB = FP2.next()
                    for c in range(8):
                        K.op("pe", lambda e, c=c: e.matmul(bk[:], lhsT=m_[:, c, t * 128:(t + 1) * 128], rhs=wo[:, c, hf * 512:(hf + 1) * 512],
                                                           start=(c == 0), stop=(c == 7)), reads=[mB], writes=[bkB])
                    K.op("dve", lambda e: e.tensor_tensor(out=xr[:, t, hf * 512:(hf + 1) * 512], in0=bk[:], in1=xr[:, t, hf * 512:(hf + 1) * 512],
                                                          op=ALU.add), reads=[bkB, xrB[t]], writes=[xrB[t]])
                rms_to_hT(t, xr[:, t, :], xrB[t], hT2, hT2B)
            for f in range(NF):
                w_, wB = WS.next()
                K.dma("sp", w_[:], wupg_s.ap()[f].rearrange("a p n -> p a n"), writes=[wB])
                ub, ubB = FP2.next()
                gb, gbB = FP2.next()
                for c in range(8):
                    K.op("pe", lambda e, c=c: e.matmul(ub[:], lhsT=w_[:, 0, c * 128:(c + 1) * 128], rhs=hT2[:, c, :],
                                                       start=(c == 0), stop=(c == 7)), reads=[wB, hT2B], writes=[ubB])
                for c in range(8):
                    K.op("pe", lambda e, c=c: e.matmul(gb[:], lhsT=w_[:, 1, c * 128:(c + 1) * 128], rhs=hT2[:, c, :],
                                                       start=(c == 0), stop=(c == 7)), reads=[wB, hT2B], writes=[gbB])
                u_, uB = usb.next()
                if j == 0:
                    hb, hbB = FP2.next()
                    for c in range(8):
                        K.op("pe", lambda e, c=c: e.matmul(hb[:, 0:2], lhsT=w_[:, 0, c * 128:(c + 1) * 128], rhs=hhT[:, c, :],
                                                           start=(c == 0), stop=(c == 7)), reads=[wB, hhTB], writes=[hbB])
                    K.op("dve", lambda e: e.tensor_scalar(out=u_[:, 0:2], in0=hb[:, 0:2], scalar1=hfl[:, 0:1], scalar2=None, op0=ALU.mult),
                         reads=[hbB], writes=[uB])
                else:
                    K.op("pool", lambda e, f=f: e.tensor_copy(out=u_[:, 0:2], in_=uh[:, f, :]), reads=[uhB[f]], writes=[uB])
                copy_op("act", u_[:, 2:514], ub[:], [ubB], [uB])
                K.op("pool", lambda e, f=f: e.tensor_copy(out=uh[:, f, :], in_=u_[:, 512:514]), reads=[uB], writes=[uhB[f]])
                y_, yB = yc.next()
                K.op("dve", lambda e, f=f: e.tensor_scalar(out=y_[:], in0=u_[:, 2:514], scalar1=cws[:, f, 2:3], scalar2=cbs[:, f:f + 1],
                                                           op0=ALU.mult, op1=ALU.add), reads=[uB], writes=[yB])
                K.op("dve", lambda e, f=f: e.scalar_tensor_tensor(out=y_[:], in0=u_[:, 1:513], scalar=cws[:, f, 1:2], in1=y_[:],
                                                                  op0=ALU.mult, op1=ALU.add), reads=[uB, yB], writes=[yB])
                K.op("dve", lambda e, f=f: e.scalar_tensor_tensor(out=y_[:], in0=u_[:, 0:512], scalar=cws[:, f, 0:1], in1=y_[:],
                                                                  op0=ALU.mult, op1=ALU.add), reads=[uB, yB], writes=[yB])
                g_, gB_ = gl.next()
                K.op("act", lambda e: e.activation(out=g_[:], in_=y_[:], func=AF.Gelu_apprx_tanh), reads=[yB], writes=[gB_])
                K.op("dve", lambda e, f=f: e.tensor_tensor(out=actT[:, f, :], in0=g_[:], in1=gb[:], op=ALU.mult),
                     reads=[gB_, gbB], writes=[actB[f]])
            for t in range(4):
                for hf in range(2):
                    bk, bkB = FP2.next()
                    for f in range(NF):
                        K.op("pe", lambda e, f=f: e.matmul(bk[:], lhsT=actT[:, f, t * 128:(t + 1) * 128], rhs=wdn[:, f, hf * 512:(hf + 1) * 512],
                                                           start=(f == 0), stop=(f == NF - 1)), reads=[actB[f]], writes=[bkB])
                    K.op("dve", lambda e: e.tensor_tensor(out=xr[:, t, hf * 512:(hf + 1) * 512], in0=bk[:], in1=xr[:, t, hf * 512:(hf + 1) * 512],
                                                          op=ALU.add), reads=[bkB, xrB[t]], writes=[xrB[t]])
            for t in range(4):
                rms_to_hT(t, xr[:, t, :], xrB[t], hT2, hT2B)
            for t in range(4):
                for hf in range(2):
                    bk, bkB = FP2.next()
                    for c in range(8):
                        K.op("pe", lambda e, c=c: e.matmul(bk[:], lhsT=hT2[:, c, t * 128:(t + 1) * 128], rhs=wg[:, c, hf * 512:(hf + 1) * 512],
                                                           start=(c == 0), stop=(c == 7)), reads=[hT2B], writes=[bkB])
                    s_, sB = sgm.next()
                    K.op("act", lambda e: e.activation(out=s_[:], in_=bk[:], func=AF.Sigmoid), reads=[bkB], writes=[sB])
                    bk2, bk2B = FP2.next()
                    for c in range(2):
                        K.op("pe", lambda e, c=c: e.matmul(bk2[:], lhsT=pT[:, c, t * 128:(t + 1) * 128], rhs=wp[:, c, hf * 512:(hf + 1) * 512],
                                                           start=(c == 0), stop=(c == 1)), reads=[pTB], writes=[bk2B])
                    K.op("dve", lambda e: e.tensor_tensor(out=s_[:], in0=bk2[:], in1=s_[:], op=ALU.mult), reads=[bk2B, sB], writes=[sB])
                    K.op("dve", lambda e: e.tensor_tensor(out=xr[:, t, hf * 512:(hf + 1) * 512], in0=s_[:], in1=xr[:, t, hf * 512:(hf + 1) * 512],
                                                          op=ALU.add), reads=[sB, xrB[t]], writes=[xrB[t]])
                ss_, ssB = ss2.next()
                K.op("act", lambda e: e.activation(out=junk2[:], in_=xr[:, t, :], func=AF.Square, accum_out=ss_[:]),
                     reads=[xrB[t]], writes=[ssB])
                K.op("act", lambda e: e.activation(out=ss_[:], in_=ss_[:], func=AF.Ln, bias=epsr[:, 0:1], scale=1.0 / D),
                     reads=[ssB], writes=[ssB])
                K.op("act", lambda e: e.activation(out=ss_[:], in_=ss_[:], func=AF.Exp, scale=-0.5), reads=[ssB], writes=[ssB])
                o_, oB = yo.next()
                K.op("dve", lambda e: e.scalar_tensor_tensor(out=o_[:], in0=xr[:, t, :], scalar=ss_[:, 0:1], in1=fins[:],
                                                             op0=ALU.mult, op1=ALU.mult), reads=[xrB[t], ssB], writes=[oB])
                r0 = j * 512 + t * 128
                K.dma("act", y[r0:r0 + 128, :], o_[:], reads=[oB])
        K.barrier()
    es.close()
    return nc


def _consts(g):
    bf = ml_dtypes.bfloat16
    out = {}
    kst = np.zeros((64, S), np.float32)
    for n in range(63):
        kst[n, n * 256:(n + 1) * 256] = 1.0
    kst[63, :] = 1.0
    out["c_kst"] = kst.astype(bf)
    slopes = [2.0 ** (-8.0 * (h + 1) / 8) for h in (g, 4 + g)]
    j = np.arange(128, dtype=np.float64)
    p = np.arange(128, dtype=np.float64)
    dtab = np.zeros((128, 2, 128), np.float64)
    negr = np.zeros((128, 2, 2), np.float64)
    ekb = np.zeros((128, 2, 2), np.float64)
    bd = np.zeros((128, 2, 2, 256), np.float64)
    r = np.arange(256, dtype=np.float64)
    for X, sl in enumerate(slopes):
        dtab[:, X, :] = (-BIG - sl * 256.0 * (64 - j))[None, :]
        for par in range(2):
            negr[:, X, par] = -sl * (par * 128 + p)
            ekb[:, X, par] = sl * (par * 128 + p)
        for c in range(2):
            t = (c * 128 + p)[:, None]
            bd[:, X, c, :] = np.where(r[None, :] >= t, -sl * (r[None, :] - t), -BIG)
    out["c_dtab"] = dtab.reshape(128, 256).astype(np.float32)
    out["c_negr"] = negr.reshape(128, 4).astype(np.float32)
    out["c_ekb"] = ekb.reshape(128, 4).astype(np.float32)
    out["c_bdiag"] = bd.reshape(128, 1024).astype(np.float32).astype(bf)
    gamma = 1.0 - 2.0 ** (-5.0 - g)
    lg = np.log(np.float64(gamma))
    dec = np.zeros((128, 2, 256), np.float64)
    for kh in range(2):
        t = (kh * 128 + p)[:, None]
        d_ = r[None, :] - t
        dec[:, kh, :] = np.where(d_ >= 0, np.exp(lg * np.maximum(d_, 0.0)), 0.0)
    out["c_decT"] = dec.reshape(128, 512).astype(np.float32)
    qd = np.exp(lg * (r + 1.0))
    out["c_qdec"] = np.tile(np.tile(qd, 2)[None, :], (128, 1)).astype(np.float32)
    kd = np.zeros((128, 2), np.float64)
    for kh in range(2):
        kd[:, kh] = np.exp(lg * (255.0 - (kh * 128 + p))) * (128.0 ** -0.5)
    out["c_kdec"] = kd.astype(np.float32)
    out["c_cd"] = np.full((128, 1), np.exp(lg * 256.0), np.float32)
    out["c_idb"] = np.eye(128, dtype=np.float32).astype(bf)
    return out


def _prep_inputs(x, p, attn_norm, w_in, ret_norm, w_out, ffn_norm, w_up, w_gate, conv_w, conv_b, w_down,
                 ple_norm, w_ple, w_ple_gate, final_norm):
    f32 = np.float32
    x = np.asarray(x, f32)
    p = np.asarray(p, f32)
    w_in0 = np.asarray(w_in, f32)[0]

    def pc(v):
        return np.ascontiguousarray(np.asarray(v, f32).reshape(8, 128).T)
    shared = {
        "an": pc(attn_norm[0]), "fn": pc(ffn_norm[0]), "pn": pc(ple_norm[0]),
        "wup": np.ascontiguousarray(np.asarray(w_up, f32)[0]), "wgate": np.ascontiguousarray(np.asarray(w_gate, f32)[0]),
        "wdown": np.ascontiguousarray(np.asarray(w_down, f32)[0]), "wple": np.ascontiguousarray(np.asarray(w_ple, f32)[0]),
        "wpg": np.ascontiguousarray(np.asarray(w_ple_gate, f32)[0]),
        "cw": np.ascontiguousarray(np.asarray(conv_w, f32)[0].reshape(3, NF, 128).transpose(2, 1, 0).reshape(128, NF * 3)),
        "cb": np.ascontiguousarray(np.asarray(conv_b, f32)[0].reshape(NF, 128).T),
        "fin": np.ascontiguousarray(np.tile(np.asarray(final_norm, f32)[None, :], (128, 1))),
    }
    wo = np.asarray(w_out, f32)[0]
    perm = []
    for gg in range(4):
        perm += (list(range(64 * gg, 64 * gg + 64)) + list(range(64 * (4 + gg), 64 * (4 + gg) + 64))
                 + list(range(512 + 128 * gg, 512 + 128 * gg + 128)))
    shared["wout"] = np.ascontiguousarray(wo[perm, :])
    rnorm = np.asarray(ret_norm, f32)[0]
    in_maps = []
    for c in range(8):
        b, g = c // 4, c % 4
        q = g
        hd = list(range(64 * g, 64 * g + 64)) + list(range(64 * (4 + g), 64 * (4 + g) + 64))
        cols_f = (hd + [512 + i for i in hd]
                  + list(range(1536 + 128 * g, 1536 + 128 * g + 128)) + list(range(2048 + 128 * g, 2048 + 128 * g + 128)))
        cols_t = ([1024 + i for i in hd] + list(range(2048 + 128 * g, 2048 + 128 * g + 128))
                  + list(range(2560 + 128 * g, 2560 + 128 * g + 128)) + list(range(3072 + 128 * g, 3072 + 128 * g + 128)))
        m = dict(shared)
        m["xb"] = x[b]
        m["xq"] = np.ascontiguousarray(x[b, q * TQ:(q + 1) * TQ])
        m["pq"] = np.ascontiguousarray(p[0, b, q * TQ:(q + 1) * TQ])
        m["xh"] = np.ascontiguousarray(x[b, q * TQ - 2:q * TQ]) if q > 0 else np.zeros((2, D), f32)
        m["hflag"] = np.full((128, 1), 1.0 if q > 0 else 0.0, f32)
        m["w1"] = np.ascontiguousarray(w_in0[:, cols_f + cols_t])
        m["rn"] = np.ascontiguousarray(np.tile(rnorm[128 * g:128 * g + 128][None, :], (128, 1)))
        m.update(_consts(g))
        in_maps.append(m)
    return in_maps


_NC_CACHE = {}


def kernel(x, p, attn_norm, w_in, ret_norm, w_out, ffn_norm, w_up, w_gate, conv_w, conv_b, w_down,
           ple_norm, w_ple, w_ple_gate, final_norm):
    in_maps = _prep_inputs(x, p, attn_norm, w_in, ret_norm, w_out, ffn_norm, w_up, w_gate, conv_w, conv_b, w_down,
                           ple_norm, w_ple, w_ple_gate, final_norm)
    nc = build_program()
    res = run_bass_kernel_spmd(nc, in_maps, core_ids=list(range(8)))
    out = np.empty((2, S, D), np.float32)
    for c in range(8):
        b, q = c // 4, c % 4
        out[b, q * TQ:(q + 1) * TQ] = np.asarray(res.results[c]["y"], np.float32)
    if DEBUG:
        DEBUG["_res"] = res
    return out
```

```python
import contextlib
import numpy as np
import ml_dtypes
import concourse.bass as bass
import concourse.mybir as mybir
from concourse.bass_utils import run_bass_kernel_spmd

F32 = mybir.dt.float32
BF16 = mybir.dt.bfloat16
ALU = mybir.AluOpType
AF = mybir.ActivationFunctionType
AX = mybir.AxisListType

S = 16384
D = 1024
NB = 64
NG = 32
NSEG = 8
SEGT = 2048
TQ = 4096
NT2 = 8
FF = 2816
NF = 22
BIG = 32768.0
WIN0 = 5
RMS_EPS = 1e-6
GN_EPS = 1e-5

DEBUG = {}


class Sem:
    def __init__(self, nc, name):
        self.h = nc.alloc_semaphore(name)
        self.v = 0


class Buf:
    __slots__ = ("name", "w", "r", "excl", "dsem")

    def __init__(self, name, excl=False):
        self.name = name
        self.w = None
        self.r = []
        self.excl = excl
        self.dsem = None


class Ctx:
    def __init__(self, nc):
        self.nc = nc
        self.eng = {"pe": nc.tensor, "act": nc.scalar, "dve": nc.vector, "pool": nc.gpsimd, "sp": nc.sync}
        self.esem = {k: Sem(nc, "e_" + k) for k in self.eng}
        self.waited = {k: {} for k in self.eng}
        self.nsem = 0
        self.dma_toks = []

    def newsem(self, name):
        self.nsem += 1
        return Sem(self.nc, "%s_%d" % (name, self.nsem))

    def wait(self, e, tok):
        sem, val = tok[0], tok[1]
        cur = self.waited[e].get(id(sem), 0)
        if cur >= val:
            return
        self.eng[e].wait_ge(sem.h, val)
        self.waited[e][id(sem)] = val

    def deps(self, e, reads, writes):
        toks = []
        for b in reads:
            if b.w is not None:
                toks.append(b.w)
            if b.excl:
                toks += [t for t in b.r if t[2] != e]
        for b in writes:
            if b.w is not None and (b.w[2] != e):
                toks.append(b.w)
            elif b.w is not None and b.excl and e != "pe":
                toks.append(b.w)
            toks += [t for t in b.r if t[2] != e]
        for t in toks:
            self.wait(e, t)

    def mark(self, tok, reads, writes):
        for b in reads:
            b.r.append(tok)
        for b in writes:
            b.w = tok
            b.r = []

    def op(self, e, fn, reads=(), writes=()):
        self.deps(e, reads, writes)
        ins = fn(self.eng[e])
        s = self.esem[e]
        s.v += 1
        ins.then_inc(s.h, 1)
        tok = (s, s.v, e)
        self.mark(tok, reads, writes)
        return tok

    def dma(self, q, out, in_, reads=(), writes=(), sem=None):
        self.deps(q, reads, writes)
        if sem is None:
            owner = None
            for b in list(writes) + list(reads):
                owner = b
                break
            if owner.dsem is None:
                owner.dsem = self.newsem("d")
            sem = owner.dsem
        ins = self.eng[q].dma_start(out=out, in_=in_)
        sem.v += 16
        ins.then_inc(sem.h, 16)
        tok = (sem, sem.v, None)
        self.mark(tok, reads, writes)
        self.dma_toks.append(tok)
        return tok

    def barrier(self):
        for e in self.eng:
            for e2, s in self.esem.items():
                if e2 != e and s.v > 0:
                    self.wait(e, (s, s.v, e2))
            for t in self.dma_toks:
                self.wait(e, t)
        self.dma_toks = []


class Rot:
    def __init__(self, items):
        self.items = items
        self.i = 0

    def next(self):
        it = self.items[self.i % len(self.items)]
        self.i += 1
        return it


def build_program():
    nc = bass.Bass("TRN2", target_bir_lowering=False)
    K = Ctx(nc)
    es = contextlib.ExitStack()

    def din(name, shape, dt=F32):
        return nc.dram_tensor(name, list(shape), dt, kind="ExternalInput").ap()

    xb = din("xb", [S, D])
    pq = din("pq", [TQ, 256])
    xq = din("xq", [TQ, D])
    xh = din("xh", [2, D])
    w1 = din("w1", [D, 1024])
    an = din("an", [128, 8])
    wout = din("wout", [D, D])
    wup = din("wup", [D, FF])
    wgate = din("wgate", [D, FF])
    wdown = din("wdown", [FF, D])
    wple = din("wple", [256, D])
    wpg = din("wpg", [D, D])
    fnorm = din("fn", [128, 8])
    pnorm = din("pn", [128, 8])
    cw = din("cw", [128, NF * 3])
    cb = din("cb", [128, NF])
    fin = din("fin", [128, D])
    rn = din("rn", [128, 128])
    hflag = din("hflag", [128, 1])
    c_kst = din("c_kst", [64, S], BF16)
    c_dtab = din("c_dtab", [128, 2 * 128])
    c_negr = din("c_negr", [128, 4])
    c_bdiag = din("c_bdiag", [128, 2 * 2 * 256], BF16)
    c_ekb = din("c_ekb", [128, 4])
    c_decT = din("c_decT", [128, 2 * 256])
    c_qdec = din("c_qdec", [128, 512])
    c_kdec = din("c_kdec", [128, 2])
    c_cd = din("c_cd", [128, 1])
    c_idb = din("c_idb", [128, 128], BF16)
    y = nc.dram_tensor("y", [TQ, D], F32, kind="ExternalOutput").ap()
    dbg = {}
    if DEBUG.get("mix"):
        dbg["mix"] = nc.dram_tensor("dbg_mix", [256, S], BF16, kind="ExternalOutput").ap()

    agi = [nc.dram_tensor("agi%d" % k, [256, SEGT], BF16) for k in range(NSEG)]
    ago = [nc.dram_tensor("ago%d" % k, [1024, SEGT], BF16) for k in range(NSEG)]
    mine = nc.dram_tensor("mine", [1024, NSEG * SEGT], BF16)
    wupg_s = nc.dram_tensor("wupg_s", [NF, 2, 128, 8 * 128], BF16)
    wdown_s = nc.dram_tensor("wdown_s", [128, NF * D], BF16)
    wout_s = nc.dram_tensor("wout_s", [128, 8 * D], BF16)
    wpg_s = nc.dram_tensor("wpg_s", [128, 8 * D], BF16)
    wple_s = nc.dram_tensor("wple_s", [128, 2 * D], BF16)

    def sb(name, shape, dt=F32):
        return es.enter_context(nc.sbuf_tensor(name, list(shape), dt))

    def ps(name, shape, dt=F32):
        return es.enter_context(nc.psum_tensor(name, list(shape), dt))

    trb = [(ps("trb%d" % i, [128, 1024], BF16), Buf("trb%d" % i, True)) for i in range(1)]
    fpb = [(ps("fpb%d" % i, [128, 512], F32), Buf("fpb%d" % i, True)) for i in range(5)]
    obk = [(ps("obk%d" % i, [128, 512], F32), Buf("obk%d" % i, True)) for i in range(2)]
    TR = Rot(trb)
    FP = Rot(fpb[0:2])
    AFP = Rot(fpb[2:5])
    FP2 = Rot(fpb)

    idb = sb("idb", [128, 128], BF16)
    epsr = sb("epsr", [128, 1])
    epsg = sb("epsg", [128, 1])
    zero2 = sb("zero2", [128, 2], BF16)
    K.dma("sp", idb[:], c_idb[:, :], writes=[Buf("c")])
    K.op("dve", lambda e: e.memset(epsr[:], RMS_EPS))
    K.op("dve", lambda e: e.memset(epsg[:], GN_EPS))
    K.op("dve", lambda e: e.memset(zero2[:], 0.0))

    gains = sb("gains", [128, 16])
    gB = Buf("gains")
    K.dma("sp", gains[:, 0:8], fnorm[:, :], writes=[gB])
    K.dma("sp", gains[:, 8:16], pnorm[:, :], writes=[gB])

    with contextlib.ExitStack() as p1:
        def s1(name, shape, dt=F32):
            return p1.enter_context(nc.sbuf_tensor(name, list(shape), dt))

        W1b = s1("W1b", [128, 8, 1024], BF16)
        KA = [s1("KaugA", [128, S], BF16), s1("KaugB", [128, S], BF16)]
        VV = s1("VV", [128, 128, 192], BF16)
        KAB = [[Buf("KaugA%d" % i) for i in range(NG)], [Buf("KaugB%d" % i) for i in range(NG)]]
        VAB = [Buf("VV%d" % i) for i in range(NG)]
        kmT = [s1("kmA", [64, 64], BF16), s1("kmB", [64, 64], BF16)]
        kmB_ = [Buf("kmA"), Buf("kmB")]
        Ctab = s1("Ctab", [128, 128])
        dtab = s1("dtab", [128, 256])
        negr = s1("negr", [128, 4])
        bdiag = s1("bdiag", [128, 2, 2, 256], BF16)
        ekb = s1("ekb", [128, 4])
        decT = s1("decT", [128, 2, 256])
        qdec = s1("qdec", [128, 512])
        kdec = s1("kdec", [128, 2])
        cdc = s1("cdc", [128, 1])
        rng_ = s1("rng", [128, 128])
        an_s = s1("an_s", [128, 8])
        cB = Buf("consts")
        for dst, src in ((dtab[:], c_dtab[:, :]), (negr[:], c_negr[:, :]),
                         (bdiag[:].rearrange("p a b c -> p (a b c)"), c_bdiag[:, :]),
                         (ekb[:], c_ekb[:, :]), (decT[:].rearrange("p a b -> p (a b)"), c_decT[:, :]),
                         (qdec[:], c_qdec[:, :]), (kdec[:], c_kdec[:, :]), (cdc[:], c_cd[:, :]),
                         (rng_[:], rn[:, :]), (an_s[:], an[:, :])):
            K.dma("sp", dst, src, writes=[cB])
        for X in range(2):
            K.dma("sp", KA[X][64:128, :], c_kst[:, :], writes=[cB])
        K.op("dve", lambda e: e.memset(Ctab[:, 0:64], 0.0))
        K.op("dve", lambda e: e.memset(Ctab[:, 64:128], -1e30))
        K.op("pool", lambda e: e.memset(VV[:, :, 64:128], 1.0))
        for X in range(2):
            K.op("dve", lambda e, X=X: e.memset(kmT[X][:], 0.0))
        with contextlib.ExitStack() as pes:
            wst = [(pes.enter_context(nc.sbuf_tensor("wst%d" % i, [128, 1024], F32)), Buf("wst%d" % i)) for i in range(2)]
            for c in range(8):
                st, stB = wst[c % 2]
                K.dma("sp", st[:], w1[c * 128:(c + 1) * 128, :], writes=[stB])
                K.op("dve", lambda e, st=st, c=c: e.tensor_scalar(out=W1b[:, c, :], in0=st[:], scalar1=an_s[:, c:c + 1],
                                                                  scalar2=None, op0=ALU.mult), reads=[stB, cB])
            K.barrier()

        mbin = [[[(s1("mbin%d%d%d" % (X, par, i), [128, 128], BF16), Buf("mbin")) for i in range(2)]
                 for par in range(2)] for X in range(2)]
        for X in range(2):
            for par in range(2):
                for i in range(2):
                    t_, _ = mbin[X][par][i]
                    K.op("dve", lambda e, t_=t_: e.memset(t_[:, 0:64], 0.0))
                    K.op("dve", lambda e, t_=t_, X=X, par=par: e.tensor_copy(out=t_[:, 127:128],
                                                                             in_=negr[:, 2 * X + par:2 * X + par + 1]))
        xt = [(s1("xt%d" % i, [128, 1024]), Buf("xt%d" % i)) for i in range(2)]
        ssb = [(s1("ss%d" % i, [128, 1]), Buf("ss")) for i in range(4)]
        rsb = [(s1("rs%d" % i, [128, 1]), Buf("rs")) for i in range(4)]
        xs = [(s1("xs%d" % i, [128, 1024], BF16), Buf("xs%d" % i)) for i in range(2)]
        hT = [(s1("hT%d" % i, [128, 8, 512], BF16), Buf("hT%d" % i)) for i in range(2)]
        QA = [[(s1("Qaug%d%d" % (X, i), [128, 512], BF16), Buf("Qaug")) for i in range(2)] for X in range(2)]
        kms = [(s1("kms%d" % i, [128, 2]), Buf("kms")) for i in range(2)]
        Gm = [(s1("Gm%d" % i, [128, 64]), Buf("Gm")) for i in range(2)]
        m8 = [(s1("m8%d" % i, [128, 8]), Buf("m8")) for i in range(2)]
        thr = [(s1("thr%d" % i, [128, 1]), Buf("thr")) for i in range(2)]
        mb32 = [(s1("mb32%d" % i, [128, 64]), Buf("mb32")) for i in range(2)]
        PT = Rot([(s1("PT%d" % i, [128, 512], BF16), Buf("PT%d" % i)) for i in range(3)])
        rc0 = (s1("rc0", [128, 512]), Buf("rc"))
        rc = [rc0, rc0]
        mixst = [(s1("mixst%d" % i, [128, 512], BF16), Buf("mixst%d" % i)) for i in range(2)]
        retst = [(s1("retst%d" % i, [128, 512], BF16), Buf("retst%d" % i)) for i in range(2)]
        rqT = [(s1("rqT%d" % i, [128, 512], BF16), Buf("rqT")) for i in range(2)]
        rqTd = [(s1("rqTd%d" % i, [128, 512], BF16), Buf("rqTd")) for i in range(2)]
        rkT = [(s1("rkT%d" % i, [128, 512], BF16), Buf("rkT")) for i in range(2)]
        rkd = [(s1("rkd%d" % i, [128, 4, 128], BF16), Buf("rkd")) for i in range(2)]
        rvt = [(s1("rvt%d" % i, [128, 4, 128], BF16), Buf("rvt")) for i in range(2)]
        sg = [(s1("sg%d" % i, [128, 4, 128]), Buf("sg")) for i in range(2)]
        gs = sg
        AT = Rot([(s1("AT%d" % i, [128, 2, 256], BF16), Buf("AT")) for i in range(2)])
        st6 = Rot([(s1("st6%d" % i, [128, 6]), Buf("st6")) for i in range(2)])
        mv = Rot([(s1("mv%d" % i, [128, 2]), Buf("mv")) for i in range(2)])
        rstd2 = Rot([(s1("rstd2%d" % i, [128, 1]), Buf("rstd2")) for i in range(2)])
        yn = Rot([(s1("yn%d" % i, [128, 128]), Buf("yn")) for i in range(2)])
        ynb = Rot([(s1("ynb%d" % i, [128, 128], BF16), Buf("ynb")) for i in range(2)])
        state = s1("state", [128, 128])
        stateB = Buf("state")
        stbf = Rot([(s1("stbf%d" % i, [128, 128], BF16), Buf("stbf")) for i in range(2)])
        K.op("dve", lambda e: e.memset(state[:], 0.0), writes=[stateB])
        cur_st = stbf.next()
        K.op("dve", lambda e: e.memset(cur_st[0][:], 0.0), writes=[cur_st[1]])
        K.barrier()

        ALT = [0]

        def evac_eng():
            ALT[0] += 1
            return "act" if ALT[0] % 2 else "dve"

        def copy_op(eng, out, in_, reads, writes, scale=None):
            if eng == "act":
                if scale is None:
                    return K.op("act", lambda e: e.copy(out=out, in_=in_), reads=reads, writes=writes)
                return K.op("act", lambda e: e.mul(out=out, in_=in_, mul=scale), reads=reads, writes=writes)
            if scale is None:
                return K.op(eng, lambda e: e.tensor_copy(out=out, in_=in_), reads=reads, writes=writes)
            return K.op(eng, lambda e: e.tensor_scalar(out=out, in0=in_, scalar1=scale, scalar2=None, op0=ALU.mult),
                        reads=reads, writes=writes)

        def prep(g):
            h, hB = hT[g % 2]
            for t in range(4):
                T = 4 * g + t
                x_, xB = xt[T % 2]
                K.dma("sp", x_[:], xb[T * 128:(T + 1) * 128, :], writes=[xB])
                ss_, ssB = ssb[T % 4]
                rs_, rsB = rsb[T % 4]
                xs_, xsB = xs[T % 2]
                K.op("act", lambda e: e.activation(out=xs_[:], in_=x_[:], func=AF.Square, accum_out=ss_[:]),
                     reads=[xB], writes=[ssB, xsB])
                K.op("act", lambda e: e.activation(out=rs_[:], in_=ss_[:], func=AF.Ln, bias=epsr[:, 0:1], scale=1.0 / D),
                     reads=[ssB], writes=[rsB])
                K.op("act", lambda e: e.activation(out=rs_[:], in_=rs_[:], func=AF.Exp, scale=-0.5), reads=[rsB], writes=[rsB])
                yield
                xs_, xsB = xs[T % 2]
                K.op("dve", lambda e: e.tensor_scalar(out=xs_[:], in0=x_[:], scalar1=rs_[:, 0:1], scalar2=None, op0=ALU.mult),
                     reads=[xB, rsB], writes=[xsB])
                yield
                tr, trB = TR.next()
                for c in range(8):
                    K.op("pe", lambda e, c=c: e.transpose(out=tr[:, c * 128:(c + 1) * 128], in_=xs_[:, c * 128:(c + 1) * 128],
                                                          identity=idb[:]), reads=[xsB], writes=[trB])
                copy_op("dve", h[:, :, t * 128:(t + 1) * 128], tr[:].rearrange("p (c j) -> p c j", j=128),
                        reads=[trB], writes=[hB])
                yield

        def proj(g, js=(0, 1, 2, 3), do_tm=True):
            h, hB = hT[g % 2]
            for j in js:
                bk, bkB = fpb[0]
                for c in range(8):
                    K.op("pe", lambda e, c=c: e.matmul(bk[:], lhsT=W1b[:, c, j * 128:(j + 1) * 128], rhs=h[:, c, :],
                                                       start=(c == 0), stop=(c == 7)), reads=[hB], writes=[bkB])
                yield
                if j == 0:
                    for X in range(2):
                        q_, qB = QA[X][g % 2]
                        copy_op("dve", q_[0:64, :], bk[64 * X:64 * X + 64, :], [bkB], [qB], scale=0.125)
                elif j == 1:
                    for X in range(2):
                        copy_op("dve", KA[X][0:64, g * 512:(g + 1) * 512], bk[64 * X:64 * X + 64, :],
                                [bkB], [KAB[X][g]])
                    km_, kmsB = kms[g % 2]
                    K.op("dve", lambda e: e.tensor_reduce(out=km_[:], in_=bk[:].rearrange("p (a b) -> p a b", b=256),
                                                          axis=AX.X, op=ALU.add), reads=[bkB], writes=[kmsB])
                    for X in range(2):
                        K.op("dve", lambda e, X=X: e.tensor_scalar(out=kmT[X][0:64, 2 * g:2 * g + 2], in0=km_[64 * X:64 * X + 64, :],
                                                                   scalar1=1.0 / 256, scalar2=None, op0=ALU.mult),
                             reads=[kmsB], writes=[kmB_[X]])
                elif j == 2:
                    copy_op("dve", rqT[g % 2][0][:], bk[:], [bkB], [rqT[g % 2][1]])
                    K.op("dve", lambda e: e.tensor_tensor(out=rqTd[g % 2][0][:], in0=bk[:], in1=qdec[:], op=ALU.mult),
                         reads=[bkB], writes=[rqTd[g % 2][1]])
                else:
                    copy_op("dve", rkT[g % 2][0][:], bk[:], [bkB], [rkT[g % 2][1]], scale=128 ** -0.5)
                yield
            for t in (range(4) if do_tm else ()):
                bk, bkB = fpb[0]
                kc = 4 * g + t
                for c in range(8):
                    K.op("pe", lambda e, c=c: e.matmul(bk[:], lhsT=h[:, c, t * 128:(t + 1) * 128], rhs=W1b[:, c, 512:1024],
                                                       start=(c == 0), stop=(c == 7)), reads=[hB], writes=[bkB])
                yield
                copy_op("dve", VV[:, kc, 0:64], bk[:, 0:64], [bkB], [VAB[g]])
                copy_op("dve", VV[:, kc, 128:192], bk[:, 64:128], [bkB], [VAB[g]])
                K.op("dve", lambda e: e.tensor_scalar(out=rkd[g % 2][0][:, t, :], in0=bk[:, 128:256],
                                                      scalar1=kdec[:, t % 2:t % 2 + 1], scalar2=None, op0=ALU.mult),
                     reads=[bkB], writes=[rkd[g % 2][1]])
                copy_op("dve", rvt[g % 2][0][:, t, :], bk[:, 256:384], [bkB], [rvt[g % 2][1]])
                K.op("act", lambda e: e.activation(out=sg[g % 2][0][:, t, :], in_=bk[:, 384:512], func=AF.Exp, scale=-1.0),
                     reads=[bkB], writes=[sg[g % 2][1]])
                K.op("dve", lambda e: e.tensor_scalar_add(out=sg[g % 2][0][:, t, :], in0=sg[g % 2][0][:, t, :], scalar1=1.0),
                     reads=[sg[g % 2][1]], writes=[sg[g % 2][1]])
                K.op("dve", lambda e: e.reciprocal(out=sg[g % 2][0][:, t, :], in_=sg[g % 2][0][:, t, :]),
                     reads=[sg[g % 2][1]], writes=[sg[g % 2][1]])
                K.op("dve", lambda e: e.tensor_tensor(out=sg[g % 2][0][:, t, :], in0=sg[g % 2][0][:, t, :], in1=bk[:, 384:512], op=ALU.mult),
                     reads=[bkB, sg[g % 2][1]], writes=[sg[g % 2][1]])
                K.op("dve", lambda e: e.tensor_tensor(out=sg[g % 2][0][:, t, :], in0=sg[g % 2][0][:, t, :], in1=rng_[:],
                                                      op=ALU.mult), reads=[sg[g % 2][1]], writes=[sg[g % 2][1]])
                yield

        MI = [0]

        def masks(g):
            bk, bkB = fpb[0]
            for t in range(4):
                for X in range(2):
                    q_, qB = QA[X][g % 2]
                    o = (4 * X + t) * 64
                    K.op("pe", lambda e: e.matmul(bk[:, o:o + 64], lhsT=q_[0:64, t * 128:(t + 1) * 128], rhs=kmT[X][0:64, 0:64],
                                                  start=True, stop=True), reads=[qB, kmB_[X]], writes=[bkB])
            yield
            mbis = []
            for t in range(4):
                a = 2 * g + t // 2
                par = t % 2
                for X in range(2):
                    o = (4 * X + t) * 64
                    i = MI[0] % 2
                    MI[0] += 1
                    K.op("dve", lambda e: e.tensor_tensor(out=Gm[i][0][:], in0=bk[:, o:o + 64], in1=Ctab[:, 64 - a:128 - a], op=ALU.add),
                         reads=[bkB], writes=[Gm[i][1]])
                    K.op("dve", lambda e: e.max(out=m8[i][0][:], in_=Gm[i][0][:]), reads=[Gm[i][1]], writes=[m8[i][1]])
                    K.op("dve", lambda e: e.tensor_scalar_max(out=thr[i][0][:], in0=m8[i][0][:, 2:3], scalar1=-1e29),
                         reads=[m8[i][1]], writes=[thr[i][1]])
                    K.op("dve", lambda e: e.tensor_scalar(out=mb32[i][0][:], in0=Gm[i][0][:], scalar1=thr[i][0][:, 0:1],
                                                          scalar2=BIG, op0=ALU.is_ge, op1=ALU.mult),
                         reads=[Gm[i][1], thr[i][1]], writes=[mb32[i][1]])
                    mbi, mbiB = mbin[X][par][(t // 2) % 2]
                    K.op("dve", lambda e: e.tensor_tensor(out=mbi[:, 64:127], in0=mb32[i][0][:, 0:63],
                                                          in1=dtab[:, 128 * X + 64 - a:128 * X + 127 - a], op=ALU.add),
                         reads=[mb32[i][1]], writes=[mbiB])
                    mbis.append((t, X, mbi, mbiB))
                    yield
            tr, trB = TR.next()
            for (t, X, mbi, mbiB) in mbis:
                o = (4 * X + t) * 128
                K.op("pe", lambda e: e.transpose(out=tr[:, o:o + 128], in_=mbi[:], identity=idb[:]), reads=[mbiB], writes=[trB])
            for X in range(2):
                q_, qB = QA[X][g % 2]
                copy_op("dve", q_[64:128, :], tr[64:128, X * 512:(X + 1) * 512], [trB], [qB])
            yield

        def attn(g, side):
            items = []
            firsts = [[True], [True]]

            def mk_pv(X, kc, lo, hi, ptref, last=False):
                ob, obB = obk[X]
                vlo = 64 * X

                def f():
                    pt, ptB = ptref[0]
                    K.op("pe", lambda e: e.matmul(ob[:, lo:hi], lhsT=VV[:, kc, vlo:vlo + 128], rhs=pt[:, lo:hi],
                                                  start=firsts[X][0], stop=last), reads=[ptB, VAB[kc // 4]], writes=[obB])
                    firsts[X][0] = False
                return f

            def past_item(X, kc, c):
                q_, qB = QA[X][g % 2]
                sref, ptref = [None], [None]

                def qk():
                    sref[0] = AFP.next()
                    sbk, sbB = sref[0]
                    K.op("pe", lambda e: e.matmul(sbk[:], lhsT=KA[X][:, kc * 128:(kc + 1) * 128], rhs=q_[:, :], start=True, stop=True),
                         reads=[qB, KAB[X][kc // 4]], writes=[sbB])

                def ex():
                    sbk, sbB = sref[0]
                    ptref[0] = PT.next()
                    pt, ptB = ptref[0]
                    K.op("act", lambda e: e.activation(out=pt[:], in_=sbk[:], func=AF.Exp, bias=ekb[:, 2 * X + c:2 * X + c + 1], scale=1.0),
                         reads=[sbB], writes=[ptB])
                return (qk, ex, mk_pv(X, kc, 0, 512, ptref))

            def diag0_item(X, c):
                q_, qB = QA[X][g % 2]
                kc = 4 * g + c
                q0 = 0 if c == 0 else 128
                sref, ptref = [None], [None]

                def qk():
                    sref[0] = AFP.next()
                    sbk, sbB = sref[0]
                    K.op("pe", lambda e: e.matmul(sbk[:, q0:256], lhsT=KA[X][0:64, kc * 128:(kc + 1) * 128], rhs=q_[0:64, q0:256],
                                                  start=True, stop=False), reads=[qB, KAB[X][kc // 4]], writes=[sbB])
                    K.op("pe", lambda e: e.matmul(sbk[:, q0:256], lhsT=idb[:], rhs=bdiag[:, X, c, q0:256], start=False, stop=True),
                         writes=[sbB])
                    K.op("pe", lambda e: e.matmul(sbk[:, 256:512], lhsT=KA[X][:, kc * 128:(kc + 1) * 128], rhs=q_[:, 256:512],
                                                  start=True, stop=True), reads=[qB, KAB[X][kc // 4]], writes=[sbB])

                def ex():
                    sbk, sbB = sref[0]
                    ptref[0] = PT.next()
                    pt, ptB = ptref[0]
                    K.op("act", lambda e: e.activation(out=pt[:, q0:256], in_=sbk[:, q0:256], func=AF.Exp), reads=[sbB], writes=[ptB])
                    K.op("act", lambda e: e.activation(out=pt[:, 256:512], in_=sbk[:, 256:512], func=AF.Exp,
                                                       bias=ekb[:, 2 * X + c:2 * X + c + 1], scale=1.0), reads=[sbB], writes=[ptB])
                return (qk, ex, mk_pv(X, kc, q0, 512, ptref))

            def diag1_item(X, c):
                q_, qB = QA[X][g % 2]
                kc = 4 * g + 2 + c
                q0 = 256 if c == 0 else 384
                sref, ptref = [None], [None]

                def qk():
                    sref[0] = AFP.next()
                    sbk, sbB = sref[0]
                    K.op("pe", lambda e: e.matmul(sbk[:, q0:512], lhsT=KA[X][0:64, kc * 128:(kc + 1) * 128], rhs=q_[0:64, q0:512],
                                                  start=True, stop=False), reads=[qB, KAB[X][kc // 4]], writes=[sbB])
                    K.op("pe", lambda e: e.matmul(sbk[:, q0:512], lhsT=idb[:], rhs=bdiag[:, X, c, q0 - 256:256], start=False, stop=True),
                         writes=[sbB])

                def ex():
                    sbk, sbB = sref[0]
                    ptref[0] = PT.next()
                    pt, ptB = ptref[0]
                    K.op("act", lambda e: e.activation(out=pt[:, q0:512], in_=sbk[:, q0:512], func=AF.Exp), reads=[sbB], writes=[ptB])
                return (qk, ex, mk_pv(X, kc, q0, 512, ptref, last=(c == 1)))

            if g == 0:
                for c in range(2):
                    for X in range(2):
                        items.append(diag0_item(X, c))
            for n in range(2 * g):
                for c in range(2):
                    for X in range(2):
                        if X == 0 and n < 2 * g - WIN0:
                            continue
                        items.append(past_item(X, 2 * n + c, c))
            if g > 0:
                for c in range(2):
                    for X in range(2):
                        items.append(diag0_item(X, c))
            for c in range(2):
                for X in range(2):
                    items.append(diag1_item(X, c))
            LA = 2
            stride = max(1, len(items) // 80)
            burst = max(1, -(-80 // len(items)))
            for i in range(len(items) + LA):
                if i < len(items):
                    items[i][0]()
                j = i - LA
                if j >= 0:
                    items[j][1]()
                    items[j][2]()
                if i % stride == 0:
                    for _ in range(burst):
                        next(side, None)
            for _ in side:
                pass
            for X in range(2):
                ob, obB = obk[X]
                r_, rB = rc[X]
                m_, mB = mixst[g % 2]
                nlo, dlo = (0, 64) if X == 0 else (64, 0)
                K.op("dve", lambda e: e.reciprocal(out=r_[nlo:nlo + 64, :], in_=ob[dlo:dlo + 64, :]), reads=[obB], writes=[rB])
                K.op("dve", lambda e: e.tensor_tensor(out=m_[nlo:nlo + 64, :], in0=ob[nlo:nlo + 64, :], in1=r_[nlo:nlo + 64, :], op=ALU.mult),
                     reads=[obB, rB], writes=[mB])

        CUR = [cur_st]

        def ret(g):
            i2 = g % 2
            rq_, rqB = rqT[i2]
            rqd_, rqdB = rqTd[i2]
            rk_, rkB = rkT[i2]
            rkd_, rkdB = rkd[i2]
            rv_, rvB = rvt[i2]
            gs_, gsB = gs[i2]
            rst, rstB = retst[i2]
            for ch in range(2):
                c0 = 256 * ch
                at, atB = AT.next()
                sbk, sbB = fpb[1]
                ub, ubB = fpb[1]
                K.op("pe", lambda e: e.matmul(sbk[:, 0:256], lhsT=rk_[:, c0:c0 + 128], rhs=rq_[:, c0:c0 + 256],
                                              start=True, stop=True), reads=[rkB, rqB], writes=[sbB])
                K.op("pe", lambda e: e.matmul(sbk[:, 256:384], lhsT=rk_[:, c0 + 128:c0 + 256], rhs=rq_[:, c0 + 128:c0 + 256],
                                              start=True, stop=True), reads=[rkB, rqB], writes=[sbB])
                for kh in range(2):
                    K.op("pe", lambda e: e.matmul(ub[:, 384:512], lhsT=rkd_[:, 2 * ch + kh, :], rhs=rv_[:, 2 * ch + kh, :],
                                                  start=(kh == 0), stop=(kh == 1)), reads=[rkdB, rvB], writes=[ubB])
                yield
                K.op("dve", lambda e: e.tensor_tensor(out=at[:, 0, 0:256], in0=sbk[:, 0:256], in1=decT[:, 0, 0:256], op=ALU.mult),
                     reads=[sbB], writes=[atB])
                K.op("dve", lambda e: e.tensor_tensor(out=at[:, 1, 128:256], in0=sbk[:, 256:384], in1=decT[:, 1, 128:256], op=ALU.mult),
                     reads=[sbB], writes=[atB])
                st_, stB_ = CUR[0]
                K.op("dve", lambda e: e.scalar_tensor_tensor(out=state[:], in0=state[:], scalar=cdc[:, 0:1], in1=ub[:, 384:512],
                                                             op0=ALU.mult, op1=ALU.add), reads=[ubB, stateB], writes=[stateB])
                nst = stbf.next()
                K.op("dve", lambda e: e.tensor_copy(out=nst[0][:], in_=state[:]), reads=[stateB], writes=[nst[1]])
                CUR[0] = nst
                yield
                rb, rbB = fpb[1]
                for qt in range(2):
                    o = 128 * qt
                    K.op("pe", lambda e: e.matmul(rb[:, o:o + 128], lhsT=at[:, 0, qt * 128:(qt + 1) * 128], rhs=rv_[:, 2 * ch, :],
                                                  start=True, stop=False), reads=[atB, rvB], writes=[rbB])
                    if qt == 1:
                        K.op("pe", lambda e: e.matmul(rb[:, o:o + 128], lhsT=at[:, 1, 128:256], rhs=rv_[:, 2 * ch + 1, :],
                                                      start=False, stop=False), reads=[atB, rvB], writes=[rbB])
                    K.op("pe", lambda e: e.matmul(rb[:, o:o + 128], lhsT=rqd_[:, c0 + qt * 128:c0 + qt * 128 + 128], rhs=st_[:],
                                                  start=False, stop=True), reads=[rqdB, stB_], writes=[rbB])
                yield
                ybs = []
                for qt in range(2):
                    o = 128 * qt
                    s6, s6B = st6.next()
                    mv_, mvB = mv.next()
                    r2, r2B = rstd2.next()
                    y_, yB = yn.next()
                    yb_, ybB = ynb.next()
                    K.op("dve", lambda e: e.bn_stats(out=s6[:], in_=rb[:, o:o + 128]), reads=[rbB], writes=[s6B])
                    K.op("dve", lambda e: e.bn_aggr(out=mv_[:], in_=s6[:]), reads=[s6B], writes=[mvB])
                    K.op("act", lambda e: e.activation(out=r2[:], in_=mv_[:, 1:2], func=AF.Ln, bias=epsg[:, 0:1], scale=1.0),
                         reads=[mvB], writes=[r2B])
                    K.op("act", lambda e: e.activation(out=r2[:], in_=r2[:], func=AF.Exp, scale=-0.5), reads=[r2B], writes=[r2B])
                    yield
                    K.op("dve", lambda e: e.tensor_scalar(out=y_[:], in0=rb[:, o:o + 128], scalar1=mv_[:, 0:1], scalar2=r2[:, 0:1],
                                                          op0=ALU.subtract, op1=ALU.mult), reads=[rbB, mvB, r2B], writes=[yB])
                    K.op("dve", lambda e: e.tensor_tensor(out=yb_[:], in0=y_[:], in1=gs_[:, 2 * ch + qt, :], op=ALU.mult),
                         reads=[yB, gsB], writes=[ybB])
                    ybs.append((yb_, ybB))
                    yield
                tr, trB = TR.next()
                for qt in range(2):
                    K.op("pe", lambda e: e.transpose(out=tr[:, qt * 128:(qt + 1) * 128], in_=ybs[qt][0][:], identity=idb[:]),
                         reads=[ybs[qt][1]], writes=[trB])
                copy_op("dve", rst[:, c0:c0 + 256], tr[:, 0:256], [trB], [rstB])
                yield

        agiB = [Buf("agi%d" % k) for k in range(NSEG)]
        agsem = [K.newsem("ag") for _ in range(NSEG)]
        mineB = Buf("mine")

        def outp(g):
            k = g // 4
            c0 = (g % 4) * 512
            m_, mB = mixst[g % 2]
            r_, rB = retst[g % 2]
            K.dma("sp", agi[k].ap()[0:128, c0:c0 + 512], m_[:], reads=[mB], writes=[agiB[k]])
            K.dma("sp", agi[k].ap()[128:256, c0:c0 + 512], r_[:], reads=[rB], writes=[agiB[k]])
            if "mix" in dbg:
                K.dma("sp", dbg["mix"][0:128, g * 512:(g + 1) * 512], m_[:], reads=[mB])
                K.dma("sp", dbg["mix"][128:256, g * 512:(g + 1) * 512], r_[:], reads=[rB])
            if g % 4 == 3:
                K.deps("pool", [agiB[k]], [])
                nc.gpsimd.collective_compute(
                    "AllGather", ALU.bypass, replica_groups=[[0, 1, 2, 3], [4, 5, 6, 7]],
                    ins=[agi[k].ap().opt()], outs=[ago[k].ap().opt()],
                ).then_inc(agsem[k].h)
                agsem[k].v = 1

        QV = [None]

        def agcopy(k):
            nc.gpsimd.wait_ge(agsem[k].h, 1)
            if QV[0] is None:
                QV[0] = nc.gpsimd.snap(nc.gpsimd.partition_id() % 4)
            qv = QV[0]
            slot = (qv * 6 + k) % 8
            K.dma("pool", mine.ap()[:, bass.ds(slot * SEGT, SEGT)], ago[k].ap()[:, :], writes=[mineB])

        import itertools
        PW = 512
        wst_f = [(s1("wcf%d" % i, [128, PW]), Buf("wcf%d" % i)) for i in range(2)]
        wst_b = [(s1("wcb%d" % i, [128, PW], BF16), Buf("wcb%d" % i)) for i in range(2)]
        WCN = [0]

        def wpiece(src, dst, dst3, gain, n=PW):
            i = WCN[0] % 2
            WCN[0] += 1
            (st, stB), (sbf, sbB) = wst_f[i], wst_b[i]
            K.dma("sp", st[:, 0:n], src, writes=[stB])
            if gain is None:
                K.op("act", lambda e: e.copy(out=sbf[:, 0:n], in_=st[:, 0:n]), reads=[stB], writes=[sbB])
            else:
                K.op("act", lambda e: e.activation(out=sbf[:, 0:n], in_=st[:, 0:n], func=AF.Identity, scale=gain),
                     reads=[stB], writes=[sbB])
            K.dma("pool", dst, sbf[:, 0:n].rearrange("p (f j) -> p f j", j=128) if dst3 else sbf[:, 0:n], reads=[sbB])

        def wconv():
            for c in range(8):
                rows = slice(c * 128, (c + 1) * 128)
                for wi, wsrc in enumerate((wup, wgate)):
                    for fo in range(0, NF, 4):
                        nf = min(4, NF - fo)
                        dst = wupg_s.ap()[fo:fo + nf, wi, :, c * 128:(c + 1) * 128].rearrange("f p j -> p f j")
                        wpiece(wsrc[rows, fo * 128:(fo + nf) * 128], dst, True, gains[:, c:c + 1], n=nf * 128)
                        yield
                for k4 in range(D // PW):
                    wpiece(wout[rows, k4 * PW:(k4 + 1) * PW], wout_s.ap()[:, c * D + k4 * PW:c * D + (k4 + 1) * PW], False, None)
                    yield
                    wpiece(wpg[rows, k4 * PW:(k4 + 1) * PW], wpg_s.ap()[:, c * D + k4 * PW:c * D + (k4 + 1) * PW], False,
                           gains[:, 8 + c:9 + c])
                    yield
            for f in range(NF):
                for k4 in range(D // PW):
                    wpiece(wdown[f * 128:(f + 1) * 128, k4 * PW:(k4 + 1) * PW],
                           wdown_s.ap()[:, f * D + k4 * PW:f * D + (k4 + 1) * PW], False, None)
                    yield
            for c in range(2):
                for k4 in range(D // PW):
                    wpiece(wple[c * 128:(c + 1) * 128, k4 * PW:(k4 + 1) * PW],
                           wple_s.ap()[:, c * D + k4 * PW:c * D + (k4 + 1) * PW], False, None)
                    yield
        WG = wconv()
        NPIECE = 8 * (2 * 6 + 4) + NF * 2 + 4
        PER_G = -(-NPIECE // NG)

        def run(gen):
            for _ in gen:
                pass

        def roundrobin(gens):
            live = list(gens)
            while live:
                for g_ in list(live):
                    if next(g_, "end") == "end":
                        live.remove(g_)
                    else:
                        yield
        run(prep(0))
        run(proj(0))
        run(masks(0))
        run(prep(1))
        for g in range(NG):
            side = [ret(g)]
            if g + 1 < NG:
                side += [itertools.chain(proj(g + 1, (0, 1), False), masks(g + 1), proj(g + 1, (2, 3), True))]
            if g + 2 < NG:
                side += [prep(g + 2)]
            side += [itertools.islice(WG, PER_G)]
            attn(g, roundrobin(side))
            outp(g)
            if g % 4 == 1 and g >= 5:
                agcopy((g - 5) // 4)
        run(WG)
        agcopy(NSEG - 1)
        K.barrier()

    with contextlib.ExitStack() as p2:
        def s2(name, shape, dt=F32):
            return p2.enter_context(nc.sbuf_tensor(name, list(shape), dt))

        wdn = s2("wdn", [128, NF, D], BF16)
        wo = s2("wo", [128, 8, D], BF16)
        wg = s2("wg", [128, 8, D], BF16)
        wp = s2("wp", [128, 2, D], BF16)
        cws = s2("cws", [128, NF, 3])
        cbs = s2("cbs", [128, NF])
        fins = s2("fins", [128, D])
        hfl = s2("hfl", [128, 1])
        cB = Buf("c2")
        K.dma("sp", wdn[:].rearrange("p f n -> p (f n)"), wdown_s.ap()[:, :], writes=[cB])
        K.dma("sp", wo[:].rearrange("p f n -> p (f n)"), wout_s.ap()[:, :], writes=[cB])
        K.dma("sp", wg[:].rearrange("p f n -> p (f n)"), wpg_s.ap()[:, :], writes=[cB])
        K.dma("sp", wp[:].rearrange("p f n -> p (f n)"), wple_s.ap()[:, :], writes=[cB])
        K.dma("sp", cws[:].rearrange("p f n -> p (f n)"), cw[:, :], writes=[cB])
        K.dma("sp", cbs[:], cb[:, :], writes=[cB])
        K.dma("sp", fins[:], fin[:, :], writes=[cB])
        K.dma("sp", hfl[:], hflag[:, :], writes=[cB])

        mixT = [(s2("mixT%d" % i, [128, 8, 512], BF16), Buf("mixT%d" % i)) for i in range(2)]
        xr = s2("xr", [128, 4, D])
        xrB = [Buf("xr%d" % t) for t in range(4)]
        pin = [(s2("pin%d" % i, [128, 256]), Buf("pin%d" % i)) for i in range(2)]
        pbf = [(s2("pbf%d" % i, [128, 256], BF16), Buf("pbf")) for i in range(2)]
        pT = s2("pT", [128, 2, 512], BF16)
        pTB = Buf("pT")
        junk2 = s2("junk2", [128, D], BF16)
        ss2 = Rot([(s2("ss2%d" % i, [128, 1]), Buf("ss2")) for i in range(4)])
        xs2 = Rot([(s2("xs2%d" % i, [128, D], BF16), Buf("xs2")) for i in range(2)])
        hT2 = s2("hT2", [128, 8, 512], BF16)
        hT2B = Buf("hT2")
        actT = s2("actT", [128, NF, 512], BF16)
        actB = [Buf("act%d" % f) for f in range(NF)]
        uh = s2("uh", [128, NF, 2])
        uhB = [Buf("uh%d" % f) for f in range(NF)]
        usb = Rot([(s2("usb%d" % i, [128, 514]), Buf("usb")) for i in range(2)])
        yc = Rot([(s2("yc%d" % i, [128, 512]), Buf("yc")) for i in range(2)])
        gl = Rot([(s2("gl%d" % i, [128, 512]), Buf("gl")) for i in range(2)])
        WS = Rot([(s2("ws%d" % i, [128, 2, 8 * 128], BF16), Buf("ws%d" % i)) for i in range(3)])
        sgm = Rot([(s2("sgm%d" % i, [128, 512]), Buf("sgm")) for i in range(2)])
        yo = Rot([(s2("yo%d" % i, [128, D]), Buf("yo%d" % i)) for i in range(2)])
        xhs = s2("xhs", [2, D])
        xhsB = Buf("xhs")
        mixh = s2("mixh", [128, 8, 2], BF16)
        mixhB = Buf("mixh")
        hhT = s2("hhT", [128, 8, 2], BF16)
        hhTB = Buf("hhT")
        xsh = s2("xsh", [2, D], BF16)
        xshB = Buf("xsh")
        ssh = s2("ssh", [2, 1])
        sshB = Buf("ssh")
        for f in range(NF):
            pass
        K.barrier()

        def rms_to_hT(t, xin_ap, xin_B, dst, dstB, npart=128):
            ss_, ssB = ss2.next()
            xs_, xsB = xs2.next()
            K.op("act", lambda e: e.activation(out=junk2[0:npart, :], in_=xin_ap, func=AF.Square, accum_out=ss_[0:npart, :]),
                 reads=[xin_B], writes=[ssB])
            K.op("act", lambda e: e.activation(out=ss_[0:npart, :], in_=ss_[0:npart, :], func=AF.Ln, bias=epsr[0:npart, 0:1], scale=1.0 / D),
                 reads=[ssB], writes=[ssB])
            K.op("act", lambda e: e.activation(out=ss_[0:npart, :], in_=ss_[0:npart, :], func=AF.Exp, scale=-0.5), reads=[ssB], writes=[ssB])
            K.op("dve", lambda e: e.tensor_scalar(out=xs_[0:npart, :], in0=xin_ap, scalar1=ss_[0:npart, 0:1], scalar2=None, op0=ALU.mult),
                 reads=[xin_B, ssB], writes=[xsB])
            tr, trB = TR.next()
            for c in range(8):
                K.op("pe", lambda e, c=c: e.transpose(out=tr[:, c * 128:c * 128 + npart], in_=xs_[0:npart, c * 128:(c + 1) * 128],
                                                      identity=idb[0:npart, 0:npart]), reads=[xsB], writes=[trB])
            if npart == 128:
                copy_op(evac_eng(), dst[:, :, t * 128:(t + 1) * 128], tr[:].rearrange("p (c j) -> p c j", j=128), [trB], [dstB])
            else:
                copy_op("dve", dst[:, :, 0:npart], tr[:].rearrange("p (c j) -> p c j", j=128)[:, :, 0:npart], [trB], [dstB])
            return ss_, ssB

        def load_mix(j, buf):
            m_, mB = buf
            seg = j // 4
            c0 = (j % 4) * 512
            K.dma("sp", m_[:], mine.ap()[:, seg * SEGT + c0:seg * SEGT + c0 + 512].rearrange("(c p) t -> p c t", p=128), reads=[mineB], writes=[mB])

        K.dma("sp", xhs[:], xh[:, :], writes=[xhsB])
        K.dma("sp", mixh[:], mine.ap()[:, 8 * SEGT - 2:8 * SEGT].rearrange("(c p) t -> p c t", p=128), reads=[mineB], writes=[mixhB])
        for hf in range(2):
            bk, bkB = FP2.next()
            for c in range(8):
                K.op("pe", lambda e, c=c: e.matmul(bk[0:2, :], lhsT=mixh[:, c, :], rhs=wo[:, c, hf * 512:(hf + 1) * 512],
                                                   start=(c == 0), stop=(c == 7)), reads=[mixhB], writes=[bkB])
            K.op("dve", lambda e: e.tensor_tensor(out=xhs[:, hf * 512:(hf + 1) * 512], in0=bk[0:2, :], in1=xhs[:, hf * 512:(hf + 1) * 512],
                                                  op=ALU.add), reads=[bkB, xhsB], writes=[xhsB])
        rms_to_hT(0, xhs[:], xhsB, hhT, hhTB, npart=2)

        load_mix(0, mixT[0])
        for j in range(NT2):
            m_, mB = mixT[j % 2]
            if j + 1 < NT2:
                load_mix(j + 1, mixT[(j + 1) % 2])
            for t in range(4):
                r0 = j * 512 + t * 128
                K.dma("sp", xr[:, t, :], xq[r0:r0 + 128, :], writes=[xrB[t]])
                p_, pB = pin[t % 2]
                K.dma("sp", p_[:], pq[r0:r0 + 128, :], writes=[pB])
                pb_, pbB = pbf[t % 2]
                K.op("pool", lambda e: e.tensor_copy(out=pb_[:], in_=p_[:]), reads=[pB], writes=[pbB])
                tr, trB = TR.next()
                for c in range(2):
                    K.op("pe", lambda e, c=c: e.transpose(out=tr[:, c * 128:(c + 1) * 128], in_=pb_[:, c * 128:(c + 1) * 128],
                                                          identity=idb[:]), reads=[pbB], writes=[trB])
                copy_op(evac_eng(), pT[:, :, t * 128:(t + 1) * 128], tr[:, 0:256].rearrange("p (c j) -> p c j", j=128), [trB], [pTB])
                for hf in range(2):
                    bk, bkB = FP2.next()
                    for c in range(8):
                        K.op("pe", lambda e, c=c: e.matmul(bk[:], lhsT=m_[:, c, t * 128:(t + 1) * 128], rhs=wo[:, c, hf * 512:(hf + 1) * 512],
                                                           start=(c == 0), stop=(c == 7)), reads=[mB], writes=[bkB])
                    K.op("dve", lambda e: e.tensor_tensor(out=xr[:, t, hf * 512:(hf + 1) * 512], in0=bk[:], in1=xr[:, t, hf * 512:(hf + 1) * 512],
                                                          op=ALU.add), reads=[bkB, xrB[t]], writes=[xrB[t]])
                rms_to_hT(t, xr[:, t, :], xrB[t], hT2, hT2B)
            for f in range(NF):
                w_, wB = WS.next()
                K.dma("sp", w_[:], wupg_s.ap()[f].rearrange("a p n -> p a n"), writes=[wB])
                ub, ubB = FP2.next()
                gb, gbB = FP2.next()
                for c in range(8):
                    K.op("pe", lambda e, c=c: e.matmul(ub[:], lhsT=w_[:, 0, c * 128:(c + 1) * 128], rhs=hT2[:, c, :],
                                                       start=(c == 0), stop=(c == 7)), reads=[wB, hT2B], writes=[ubB])
                for c in range(8):
                    K.op("pe", lambda e, c=c: e.matmul(gb[:], lhsT=w_[:, 1, c * 128:(c + 1) * 128], rhs=hT2[:, c, :],
                                                       start=(c == 0), stop=(c == 7)), reads=[wB, hT2B], writes=[gbB])
                u_, uB = usb.next()
                if j == 0:
                    hb, hbB = FP2.next()
                    for c in range(8):
                        K.op("pe", lambda e, c=c: e.matmul(hb[:, 0:2], lhsT=w_[:, 0, c * 128:(c + 1) * 128], rhs=hhT[:, c, :],
                                                           start=(c == 0), stop=(c == 7)), reads=[wB, hhTB], writes=[hbB])
                    K.op("dve", lambda e: e.tensor_scalar(out=u_[:, 0:2], in0=hb[:, 0:2], scalar1=hfl[:, 0:1], scalar2=None, op0=ALU.mult),
                         reads=[hbB], writes=[uB])
                else:
                    K.op("pool", lambda e, f=f: e.tensor_copy(out=u_[:, 0:2], in_=uh[:, f, :]), reads=[uhB[f]], writes=[uB])
                copy_op("act", u_[:, 2:514], ub[:], [ubB], [uB])
                K.op("pool", lambda e, f=f: e.tensor_copy(out=uh[:, f, :], in_=u_[:, 512:514]), reads=[uB], writes=[uhB[f]])
                y_, yB = yc.next()
                K.op("dve", lambda e, f=f: e.tensor_scalar(out=y_[:], in0=u_[:, 2:514], scalar1=cws[:, f, 2:3], scalar2=cbs[:, f:f + 1],
                                                           op0=ALU.mult, op1=ALU.add), reads=[uB], writes=[yB])
                K.op("dve", lambda e, f=f: e.scalar_tensor_tensor(out=y_[:], in0=u_[:, 1:513], scalar=cws[:, f, 1:2], in1=y_[:],
                                                                  op0=ALU.mult, op1=ALU.add), reads=[uB, yB], writes=[yB])
                K.op("dve", lambda e, f=f: e.scalar_tensor_tensor(out=y_[:], in0=u_[:, 0:512], scalar=cws[:, f, 0:1], in1=y_[:],
                                                                  op0=ALU.mult, op1=ALU.add), reads=[uB, yB], writes=[yB])
                g_, gB_ = gl.next()
                K.op("act", lambda e: e.activation(out=g_[:], in_=y_[:], func=AF.Gelu_apprx_tanh), reads=[yB], writes=[gB_])
                K.op("dve", lambda e, f=f: e.tensor_tensor(out=actT[:, f, :], in0=g_[:], in1=gb[:], op=ALU.mult),
                     reads=[gB_, gbB], writes=[actB[f]])
            for t in range(4):
                for hf in range(2):
                    bk, bkB = FP2.next()
                    for f in range(NF):
                        K.op("pe", lambda e, f=f: e.matmul(bk[:], lhsT=actT[:, f, t * 128:(t + 1) * 128], rhs=wdn[:, f, hf * 512:(hf + 1) * 512],
                                                           start=(f == 0), stop=(f == NF - 1)), reads=[actB[f]], writes=[bkB])
                    K.op("dve", lambda e: e.tensor_tensor(out=xr[:, t, hf * 512:(hf + 1) * 512], in0=bk[:], in1=xr[:, t, hf * 512:(hf + 1) * 512],
                                                          op=ALU.add), reads=[bkB, xrB[t]], writes=[xrB[t]])
            for t in range(4):
                rms_to_hT(t, xr[:, t, :], xrB[t], hT2, hT2B)
            for t in range(4):
                for hf in range(2):
                    bk, bkB = FP2.next()
                    for c in range(8):
                        K.op("pe", lambda e, c=c: e.matmul(bk[:], lhsT=hT2[:, c, t * 128:(t + 1) * 128], rhs=wg[:, c, hf * 512:(hf + 1) * 512],
                                                           start=(c == 0), stop=(c == 7)), reads=[hT2B], writes=[bkB])
                    s_, sB = sgm.next()
                    K.op("act", lambda e: e.activation(out=s_[:], in_=bk[:], func=AF.Sigmoid), reads=[bkB], writes=[sB])
                    bk2, bk2B = FP2.next()
                    for c in range(2):
                        K.op("pe", lambda e, c=c: e.matmul(bk2[:], lhsT=pT[:, c, t * 128:(t + 1) * 128], rhs=wp[:, c, hf * 512:(hf + 1) * 512],
                                                           start=(c == 0), stop=(c == 1)), reads=[pTB], writes=[bk2B])
                    K.op("dve", lambda e: e.tensor_tensor(out=s_[:], in0=bk2[:], in1=s_[:], op=ALU.mult), reads=[bk2B, sB], writes=[sB])
                    K.op("dve", lambda e: e.tensor_tensor(out=xr[:, t, hf * 512:(hf + 1) * 512], in0=s_[:], in1=xr[:, t, hf * 512:(hf + 1) * 512],
                                                          op=ALU.add), reads=[sB, xrB[t]], writes=[xrB[t]])
                ss_, ssB = ss2.next()
                K.op("act", lambda e: e.activation(out=junk2[:], in_=xr[:, t, :], func=AF.Square, accum_out=ss_[:]),
                     reads=[xrB[t]], writes=[ssB])
                K.op("act", lambda e: e.activation(out=ss_[:], in_=ss_[:], func=AF.Ln, bias=epsr[:, 0:1], scale=1.0 / D),
                     reads=[ssB], writes=[ssB])
                K.op("act", lambda e: e.activation(out=ss_[:], in_=ss_[:], func=AF.Exp, scale=-0.5), reads=[ssB], writes=[ssB])
                o_, oB = yo.next()
                K.op("dve", lambda e: e.scalar_tensor_tensor(out=o_[:], in0=xr[:, t, :], scalar=ss_[:, 0:1], in1=fins[:],
                                                             op0=ALU.mult, op1=ALU.mult), reads=[xrB[t], ssB], writes=[oB])
                r0 = j * 512 + t * 128
                K.dma("act", y[r0:r0 + 128, :], o_[:], reads=[oB])
        K.barrier()
    es.close()
    return nc


def _consts(g):
    bf = ml_dtypes.bfloat16
    out = {}
    kst = np.zeros((64, S), np.float32)
    for n in range(63):
        kst[n, n * 256:(n + 1) * 256] = 1.0
    kst[63, :] = 1.0
    out["c_kst"] = kst.astype(bf)
    slopes = [2.0 ** (-8.0 * (h + 1) / 8) for h in (g, 4 + g)]
    j = np.arange(128, dtype=np.float64)
    p = np.arange(128, dtype=np.float64)
    dtab = np.zeros((128, 2, 128), np.float64)
    negr = np.zeros((128, 2, 2), np.float64)
    ekb = np.zeros((128, 2, 2), np.float64)
    bd = np.zeros((128, 2, 2, 256), np.float64)
    r = np.arange(256, dtype=np.float64)
    for X, sl in enumerate(slopes):
        dtab[:, X, :] = (-BIG - sl * 256.0 * (64 - j))[None, :]
        for par in range(2):
            negr[:, X, par] = -sl * (par * 128 + p)
            ekb[:, X, par] = sl * (par * 128 + p)
        for c in range(2):
            t = (c * 128 + p)[:, None]
            bd[:, X, c, :] = np.where(r[None, :] >= t, -sl * (r[None, :] - t), -BIG)
    out["c_dtab"] = dtab.reshape(128, 256).astype(np.float32)
    out["c_negr"] = negr.reshape(128, 4).astype(np.float32)
    out["c_ekb"] = ekb.reshape(128, 4).astype(np.float32)
    out["c_bdiag"] = bd.reshape(128, 1024).astype(np.float32).astype(bf)
    gamma = 1.0 - 2.0 ** (-5.0 - g)
    lg = np.log(np.float64(gamma))
    dec = np.zeros((128, 2, 256), np.float64)
    for kh in range(2):
        t = (kh * 128 + p)[:, None]
        d_ = r[None, :] - t
        dec[:, kh, :] = np.where(d_ >= 0, np.exp(lg * np.maximum(d_, 0.0)), 0.0)
    out["c_decT"] = dec.reshape(128, 512).astype(np.float32)
    qd = np.exp(lg * (r + 1.0))
    out["c_qdec"] = np.tile(np.tile(qd, 2)[None, :], (128, 1)).astype(np.float32)
    kd = np.zeros((128, 2), np.float64)
    for kh in range(2):
        kd[:, kh] = np.exp(lg * (255.0 - (kh * 128 + p))) * (128.0 ** -0.5)
    out["c_kdec"] = kd.astype(np.float32)
    out["c_cd"] = np.full((128, 1), np.exp(lg * 256.0), np.float32)
    out["c_idb"] = np.eye(128, dtype=np.float32).astype(bf)
    return out


def _prep_inputs(x, p, attn_norm, w_in, ret_norm, w_out, ffn_norm, w_up, w_gate, conv_w, conv_b, w_down,
                 ple_norm, w_ple, w_ple_gate, final_norm):
    f32 = np.float32
    x = np.asarray(x, f32)
    p = np.asarray(p, f32)
    w_in0 = np.asarray(w_in, f32)[0]

    def pc(v):
        return np.ascontiguousarray(np.asarray(v, f32).reshape(8, 128).T)
    shared = {
        "an": pc(attn_norm[0]), "fn": pc(ffn_norm[0]), "pn": pc(ple_norm[0]),
        "wup": np.ascontiguousarray(np.asarray(w_up, f32)[0]), "wgate": np.ascontiguousarray(np.asarray(w_gate, f32)[0]),
        "wdown": np.ascontiguousarray(np.asarray(w_down, f32)[0]), "wple": np.ascontiguousarray(np.asarray(w_ple, f32)[0]),
        "wpg": np.ascontiguousarray(np.asarray(w_ple_gate, f32)[0]),
        "cw": np.ascontiguousarray(np.asarray(conv_w, f32)[0].reshape(3, NF, 128).transpose(2, 1, 0).reshape(128, NF * 3)),
        "cb": np.ascontiguousarray(np.asarray(conv_b, f32)[0].reshape(NF, 128).T),
        "fin": np.ascontiguousarray(np.tile(np.asarray(final_norm, f32)[None, :], (128, 1))),
    }
    wo = np.asarray(w_out, f32)[0]
    perm = []
    for gg in range(4):
        perm += (list(range(64 * gg, 64 * gg + 64)) + list(range(64 * (4 + gg), 64 * (4 + gg) + 64))
                 + list(range(512 + 128 * gg, 512 + 128 * gg + 128)))
    shared["wout"] = np.ascontiguousarray(wo[perm, :])
    rnorm = np.asarray(ret_norm, f32)[0]
    in_maps = []
    for c in range(8):
        b, g = c // 4, c % 4
        q = g
        hd = list(range(64 * g, 64 * g + 64)) + list(range(64 * (4 + g), 64 * (4 + g) + 64))
        cols_f = (hd + [512 + i for i in hd]
                  + list(range(1536 + 128 * g, 1536 + 128 * g + 128)) + list(range(2048 + 128 * g, 2048 + 128 * g + 128)))
        cols_t = ([1024 + i for i in hd] + list(range(2048 + 128 * g, 2048 + 128 * g + 128))
                  + list(range(2560 + 128 * g, 2560 + 128 * g + 128)) + list(range(3072 + 128 * g, 3072 + 128 * g + 128)))
        m = dict(shared)
        m["xb"] = x[b]
        m["xq"] = np.ascontiguousarray(x[b, q * TQ:(q + 1) * TQ])
        m["pq"] = np.ascontiguousarray(p[0, b, q * TQ:(q + 1) * TQ])
        m["xh"] = np.ascontiguousarray(x[b, q * TQ - 2:q * TQ]) if q > 0 else np.zeros((2, D), f32)
        m["hflag"] = np.full((128, 1), 1.0 if q > 0 else 0.0, f32)
        m["w1"] = np.ascontiguousarray(w_in0[:, cols_f + cols_t])
        m["rn"] = np.ascontiguousarray(np.tile(rnorm[128 * g:128 * g + 128][None, :], (128, 1)))
        m.update(_consts(g))
        in_maps.append(m)
    return in_maps


_NC_CACHE = {}


def kernel(x, p, attn_norm, w_in, ret_norm, w_out, ffn_norm, w_up, w_gate, conv_w, conv_b, w_down,
           ple_norm, w_ple, w_ple_gate, final_norm):
    in_maps = _prep_inputs(x, p, attn_norm, w_in, ret_norm, w_out, ffn_norm, w_up, w_gate, conv_w, conv_b, w_down,
                           ple_norm, w_ple, w_ple_gate, final_norm)
    nc = build_program()
    res = run_bass_kernel_spmd(nc, in_maps, core_ids=list(range(8)))
    out = np.empty((2, S, D), np.float32)
    for c in range(8):
        b, q = c // 4, c % 4
        out[b, q * TQ:(q + 1) * TQ] = np.asarray(res.results[c]["y"], np.float32)
    if DEBUG:
        DEBUG["_res"] = res
    return out
```
